# Optimizing a Trainium2 kernel written in Bass

```python
import jax, jax.numpy as jnp
from jax import lax
import numpy as np

D_MODEL = 1024
BATCH = 16
SEQ = 2048
DEPTH = 4

N_A_LAYERS = DEPTH // 2
N_B_LAYERS = DEPTH - N_A_LAYERS
D_FF = 4 * D_MODEL
NORM_EPS = 1e-6
CHUNK = 128
SGU_WIDTH = D_MODEL
SGU_GROUPS = 8
SGU_GROUP_DIM = SGU_WIDTH // SGU_GROUPS
N_HEADS = 16
N_KV_HEADS = 4
HEADS_PER_GROUP = N_HEADS // N_KV_HEADS
HEAD_DIM = D_MODEL // N_HEADS
CMP_BLOCK = 32
CMP_STRIDE = 16
CMP_HIDDEN = 4 * HEAD_DIM
SEL_BLOCK = 64
N_SELECT = 16
WINDOW = 512
Q_BLOCK = 16
N_BRANCH = 3
FORCE_SCORE = 1e4

kernel_name = 'yoco_gmlp_nsa_hybrid'


def rms_norm(x, g):
    xf = x.astype(jnp.float32)
    y = xf * lax.rsqrt(jnp.mean(xf * xf, axis=-1, keepdims=True) + NORM_EPS)
    return (y * g.astype(jnp.float32)).astype(x.dtype)


def layer_norm(x, g, b):
    xf = x.astype(jnp.float32)
    mu = jnp.mean(xf, axis=-1, keepdims=True)
    var = jnp.mean(jnp.square(xf - mu), axis=-1, keepdims=True)
    y = (xf - mu) * lax.rsqrt(var + NORM_EPS) * g.astype(jnp.float32) + b.astype(jnp.float32)
    return y.astype(x.dtype)


def modulate(h, shift, scale):
    return h * (1.0 + scale[:, None, :]) + shift[:, None, :]


def masked_softmax(s, mask):
    s = jnp.where(mask, s.astype(jnp.float32), -jnp.inf)
    m = jnp.max(s, axis=-1, keepdims=True)
    m = jnp.where(jnp.isfinite(m), m, 0.0)
    e = jnp.exp(s - m)
    den = jnp.sum(e, axis=-1, keepdims=True)
    return e / jnp.where(den > 0, den, 1.0)


def gmlp_mixer(h, w_in, ln_g, ln_b, w_s, b_s, w_out):
    B, T, _ = h.shape
    z = jax.nn.gelu(h @ w_in)
    u, v = jnp.split(z, 2, axis=-1)
    v = layer_norm(v, ln_g, ln_b)
    v = v.reshape(B, T // CHUNK, CHUNK, SGU_GROUPS, SGU_GROUP_DIM)
    causal = jnp.tril(jnp.ones((CHUNK, CHUNK), dtype=bool))
    w = jnp.where(causal[None], w_s, 0.0).astype(v.dtype)
    mixed = jnp.einsum('gts,bcsgd->bctgd', w, v) + b_s.T[:, :, None]
    out = u * mixed.reshape(B, T, SGU_WIDTH)
    return out @ w_out


def nsa_shared_kv(x, c_act, ada_w, ada_b, norm_g, w_kv, cmp_pos, cmp_w1, cmp_b1, cmp_w2, cmp_b2):
    B, T, _ = x.shape
    shift, scale = jnp.split(c_act @ ada_w + ada_b, 2, axis=-1)
    h = modulate(rms_norm(x, norm_g), shift, scale)
    kv = (h @ w_kv).reshape(B, T, 2 * N_BRANCH, N_KV_HEADS, HEAD_DIM)
    k_cmp_raw, v_cmp_raw = kv[:, :, 0], kv[:, :, 1]
    k_sel, v_sel = kv[:, :, 2], kv[:, :, 3]
    k_win, v_win = kv[:, :, 4], kv[:, :, 5]
    n_cmp = (T - CMP_BLOCK) // CMP_STRIDE + 1
    tok = jnp.arange(n_cmp)[:, None] * CMP_STRIDE + jnp.arange(CMP_BLOCK)[None, :]

    def compress(raw, j):
        blk = raw[:, tok] + cmp_pos[j][None, None, :, None, :]
        blk = blk.transpose(0, 1, 3, 2, 4).reshape(B, n_cmp, N_KV_HEADS, CMP_BLOCK * HEAD_DIM)
        hid = jax.nn.gelu(blk @ cmp_w1[j] + cmp_b1[j])
        return hid @ cmp_w2[j] + cmp_b2[j]

    n_sel = T // SEL_BLOCK

    def to_blocks(a):
        return a.reshape(B, n_sel, SEL_BLOCK, N_KV_HEADS, HEAD_DIM).transpose(0, 3, 1, 2, 4)

    pad = ((0, 0), (WINDOW, 0), (0, 0), (0, 0))
    return (compress(k_cmp_raw, 0), compress(v_cmp_raw, 1), to_blocks(k_sel), to_blocks(v_sel),
            jnp.pad(k_win, pad), jnp.pad(v_win, pad))


def nsa_mixer(h, w_in, w_out, k_cmp, v_cmp, k_sel, v_sel, k_win, v_win):
    B, T, _ = h.shape
    HD = N_HEADS * HEAD_DIM
    proj = h @ w_in
    q = proj[..., :HD].reshape(B, T, N_KV_HEADS, HEADS_PER_GROUP, HEAD_DIM) * (HEAD_DIM ** -0.5)
    gates = jax.nn.sigmoid(proj[..., HD:].astype(jnp.float32)).reshape(
        B, T, N_KV_HEADS, HEADS_PER_GROUP, N_BRANCH)
    pos = jnp.arange(T)

    n_cmp = k_cmp.shape[1]
    cmp_end = jnp.arange(n_cmp) * CMP_STRIDE + CMP_BLOCK - 1
    s_cmp = jnp.einsum('btghd,bcgd->bghtc', q, k_cmp)
    p_cmp = masked_softmax(s_cmp, cmp_end[None, :] <= pos[:, None])
    o_cmp = jnp.einsum('bghtc,bcgd->btghd', p_cmp.astype(v_cmp.dtype), v_cmp)

    n_blocks = k_sel.shape[2]
    ci = jnp.arange(n_cmp)[:, None] * CMP_STRIDE
    sj = jnp.arange(n_blocks)[None, :] * SEL_BLOCK
    overlap = ((ci < sj + SEL_BLOCK) & (ci + CMP_BLOCK > sj)).astype(jnp.float32)
    imp = jnp.einsum('bghtc,cj->bgtj', p_cmp, overlap)
    cur = (pos // SEL_BLOCK)[:, None]
    js = jnp.arange(n_blocks)[None, :]
    imp = jnp.where(js > cur, -1.0, imp)
    forced = (js == 0) | (js == cur) | (js == cur - 1)
    imp = jnp.where(forced, FORCE_SCORE, imp)
    n_top = min(N_SELECT, n_blocks)
    _, sel_idx = lax.top_k(imp, n_top)

    bi = jnp.arange(B)[:, None, None, None]
    gi = jnp.arange(N_KV_HEADS)[None, :, None, None]
    in_blk = jnp.arange(SEL_BLOCK)
    win_off = jnp.arange(WINDOW + Q_BLOCK)

    def block_step(start):
        tq = start + jnp.arange(Q_BLOCK)
        qb = lax.dynamic_slice_in_dim(q, start, Q_BLOCK, axis=1)
        gb = lax.dynamic_slice_in_dim(gates, start, Q_BLOCK, axis=1)
        ocb = lax.dynamic_slice_in_dim(o_cmp, start, Q_BLOCK, axis=1)
        ib = lax.dynamic_slice_in_dim(sel_idx, start, Q_BLOCK, axis=2)
        ks = k_sel[bi, gi, ib].reshape(B, N_KV_HEADS, Q_BLOCK, n_top * SEL_BLOCK, HEAD_DIM)
        vs = v_sel[bi, gi, ib].reshape(B, N_KV_HEADS, Q_BLOCK, n_top * SEL_BLOCK, HEAD_DIM)
        kpos = (ib[..., None] * SEL_BLOCK + in_blk).reshape(B, N_KV_HEADS, 1, Q_BLOCK, n_top * SEL_BLOCK)
        s_sel = jnp.einsum('bqghd,bgqmd->bghqm', qb, ks)
        p_sel = masked_softmax(s_sel, kpos <= tq[None, None, None, :, None])
        o_sel = jnp.einsum('bghqm,bgqmd->bqghd', p_sel.astype(vs.dtype), vs)
        kw = lax.dynamic_slice_in_dim(k_win, start, WINDOW + Q_BLOCK, axis=1)
        vw = lax.dynamic_slice_in_dim(v_win, start, WINDOW + Q_BLOCK, axis=1)
        kpos_w = start - WINDOW + win_off
        dist = tq[:, None] - kpos_w[None, :]
        mask_w = (dist >= 0) & (dist < WINDOW) & (kpos_w[None, :] >= 0)
        s_w = jnp.einsum('bqghd,bkgd->bghqk', qb, kw)
        p_w = masked_softmax(s_w, mask_w)
        o_win = jnp.einsum('bghqk,bkgd->bqghd', p_w.astype(vw.dtype), vw)
        o = gb[..., 0:1] * ocb + gb[..., 1:2] * o_sel + gb[..., 2:3] * o_win
        return o.reshape(B, Q_BLOCK, HD).astype(h.dtype)

    starts = jnp.arange(T // Q_BLOCK) * Q_BLOCK
    o = lax.map(block_step, starts)
    o = o.transpose(1, 0, 2, 3).reshape(B, T, HD)
    return o @ w_out


def setup_inputs(seed: int = 0) -> dict:
    key = jax.random.key(seed)
    ks = jax.random.split(key, 32)
    f32 = jnp.float32
    D = D_MODEL
    n = lambda k, shape, s: jax.random.normal(k, shape, f32) * s
    HD = N_HEADS * HEAD_DIM
    return {
        'x': n(ks[0], (BATCH, SEQ, D), 1.0),
        'c': n(ks[1], (BATCH, D), 1.0),
        'ada_w': n(ks[2], (DEPTH, D, 6 * D), D ** -0.5),
        'ada_b': n(ks[3], (DEPTH, 6 * D), 0.05),
        'norm_g': 1.0 + n(ks[4], (DEPTH, 4, D), 0.1),
        'a_w_in': n(ks[5], (N_A_LAYERS, D, 2 * SGU_WIDTH), D ** -0.5),
        'a_ln_g': 1.0 + n(ks[6], (N_A_LAYERS, SGU_WIDTH), 0.1),
        'a_ln_b': n(ks[7], (N_A_LAYERS, SGU_WIDTH), 0.05),
        'a_w_s': n(ks[8], (N_A_LAYERS, SGU_GROUPS, CHUNK, CHUNK), CHUNK ** -0.5),
        'a_b_s': 1.0 + n(ks[9], (N_A_LAYERS, SGU_GROUPS, CHUNK), 0.1),
        'a_w_out': n(ks[10], (N_A_LAYERS, SGU_WIDTH, D), SGU_WIDTH ** -0.5),
        'kv_ada_w': n(ks[11], (D, 2 * D), D ** -0.5),
        'kv_ada_b': n(ks[12], (2 * D,), 0.05),
        'kv_norm_g': 1.0 + n(ks[13], (D,), 0.1),
        'kv_w': n(ks[14], (D, 2 * N_BRANCH * N_KV_HEADS * HEAD_DIM), D ** -0.5),
        'cmp_pos': n(ks[15], (2, CMP_BLOCK, HEAD_DIM), 0.5),
        'cmp_w1': n(ks[16], (2, CMP_BLOCK * HEAD_DIM, CMP_HIDDEN), (CMP_BLOCK * HEAD_DIM) ** -0.5),
        'cmp_b1': n(ks[17], (2, CMP_HIDDEN), 0.05),
        'cmp_w2': n(ks[18], (2, CMP_HIDDEN, HEAD_DIM), CMP_HIDDEN ** -0.5),
        'cmp_b2': n(ks[19], (2, HEAD_DIM), 0.05),
        'b_w_in': n(ks[20], (N_B_LAYERS, D, HD + N_BRANCH * N_HEADS), D ** -0.5),
        'b_w_out': n(ks[21], (N_B_LAYERS, HD, D), HD ** -0.5),
        'ff_w_in': n(ks[22], (DEPTH, D, D_FF), D ** -0.5),
        'ff_w_out': n(ks[23], (DEPTH, D_FF, D), D_FF ** -0.5),
    }


def reference(x, c, ada_w, ada_b, norm_g, a_w_in, a_ln_g, a_ln_b, a_w_s, a_b_s, a_w_out,
              kv_ada_w, kv_ada_b, kv_norm_g, kv_w, cmp_pos, cmp_w1, cmp_b1, cmp_w2, cmp_b2,
              b_w_in, b_w_out, ff_w_in, ff_w_out):
    c_act = jax.nn.silu(c)
    shared = None
    for layer in range(DEPTH):
        mod = c_act @ ada_w[layer] + ada_b[layer]
        sh1, sc1, g1, sh2, sc2, g2 = jnp.split(mod, 6, axis=-1)
        h = modulate(rms_norm(x, norm_g[layer, 0]), sh1, sc1)
        if layer < N_A_LAYERS:
            y = gmlp_mixer(h, a_w_in[layer], a_ln_g[layer], a_ln_b[layer], a_w_s[layer],
                           a_b_s[layer], a_w_out[layer])
        else:
            if layer == N_A_LAYERS:
                shared = nsa_shared_kv(x, c_act, kv_ada_w, kv_ada_b, kv_norm_g, kv_w,
                                       cmp_pos, cmp_w1, cmp_b1, cmp_w2, cmp_b2)
            j = layer - N_A_LAYERS
            y = nsa_mixer(h, b_w_in[j], b_w_out[j], *shared)
        x = x + g1[:, None, :] * rms_norm(y, norm_g[layer, 1])
        h = modulate(rms_norm(x, norm_g[layer, 2]), sh2, sc2)
        y = jnp.square(jax.nn.relu(h @ ff_w_in[layer])) @ ff_w_out[layer]
        x = x + g2[:, None, :] * rms_norm(y, norm_g[layer, 3])
    return x
```

```python
import numpy as np
import concourse.bass as bass
import concourse.mybir as mybir
from concourse.bass_utils import run_bass_kernel_spmd

F32 = mybir.dt.float32
BF16 = mybir.dt.bfloat16
AF = mybir.ActivationFunctionType
ALU = mybir.AluOpType

NCORES = 8
D = 1024
T = 2048
TT = 512
NTILE = T // TT
BPC = 2
DFF = 4096
EPS = 1e-6
NEG = -30000.0


class Tl:
    __slots__ = ("w", "r")

    def __init__(self):
        self.w = None
        self.r = {}

    def addr(self, tok):
        k = id(tok[0])
        o = self.r.get(k)
        if o is None or o[1] < tok[1]:
            self.r[k] = tok


class Prog:
    ENG = ("pe", "act", "dve", "pool", "sp")

    def __init__(self, nc):
        self.nc = nc
        self.ops = {e: [] for e in self.ENG}
        self.cnt = {e: 0 for e in self.ENG}
        self.esem = {e: nc.alloc_semaphore("e_" + e) for e in self.ENG}
        self.waited = {e: {} for e in self.ENG}
        self.dcnt = {}
        self.nsem = 0

    def new_sem(self, name):
        self.nsem += 1
        return self.nc.alloc_semaphore("%s_%d" % (name, self.nsem))

    def _deps(self, eng, reads, writes):
        best = {}

        def add(tok):
            s, v = tok
            k = id(s)
            if k not in best or best[k][1] < v:
                best[k] = tok

        for t in reads:
            if t.w is not None:
                add(t.w)
        for t in writes:
            if t.w is not None:
                add(t.w)
            for tok in t.r.values():
                add(tok)
        out = []
        wd = self.waited[eng]
        pes = id(self.esem["pe"])
        for k, (s, v) in best.items():
            if eng == "pe" and k == pes:
                continue
            if wd.get(k, 0) >= v:
                continue
            wd[k] = v
            out.append((s, v))
        return out

    def op(self, eng, fn, reads=(), writes=(), dma_sem=None):
        waits = self._deps(eng, reads, writes)
        if dma_sem is None:
            self.cnt[eng] += 1
            tok = (self.esem[eng], self.cnt[eng])
            inc = (self.esem[eng], 1)
        else:
            k = id(dma_sem)
            self.dcnt[k] = self.dcnt.get(k, 0) + 16
            tok = (dma_sem, self.dcnt[k])
            inc = (dma_sem, 16)
        self.ops[eng].append((fn, waits, inc))
        for t in reads:
            t.addr(tok)
        for t in writes:
            t.w = tok
            t.r = {}
        return tok

    def handover(self, old, new):
        toks = []
        for t in old:
            if t.w is not None:
                toks.append(t.w)
            toks.extend(t.r.values())
        best = {}
        for (s, v) in toks:
            k = id(s)
            if k not in best or best[k][1] < v:
                best[k] = (s, v)
        for t in new:
            for tok in best.values():
                t.addr(tok)

    def emit(self, final_waits):
        nc = self.nc
        engmap = {"pe": "tensor", "act": "scalar", "dve": "vector", "pool": "gpsimd", "sp": "sync"}
        with nc.Block() as block:
            for e in self.ENG:
                ops = self.ops[e]

                def run(engobj, ops=ops, e=e):
                    for (fn, waits, inc) in ops:
                        for (s, v) in waits:
                            engobj.wait_ge(s, v)
                        r = fn(engobj)
                        r.then_inc(inc[0], inc[1])
                    if e == "sp":
                        for (s, v) in final_waits:
                            engobj.wait_ge(s, v)

                getattr(block, engmap[e])(run)


class Buf:
    def __init__(self, t):
        self.t = t
        self.d = Tl()


def build_program(n_layers=4):
    nc = bass.Bass("TRN2", target_bir_lowering=False)
    P = Prog(nc)

    def din(name, shape, dt=F32):
        return nc.dram_tensor(name, list(shape), dt, kind="ExternalInput").ap()

    x_d = din("x", [BPC, T, D])
    out_d = nc.dram_tensor("out", [BPC, T, D], F32, kind="ExternalOutput").ap()
    cT_d = din("cT", [128, 8, BPC])
    ada_w_d = din("ada_w", [4, D, 6 * D])
    ada_b_d = din("ada_bT", [128, 4 * 48])
    norm_g_d = din("norm_gT", [128, 16 * 8])
    a_w_in_d = din("a_w_in", [2, D, 2 * D])
    a_ln_g_d = din("a_ln_g", [2, D])
    a_ln_b_d = din("a_ln_bT", [128, 2 * 8])
    a_w_s_d = din("a_w_s", [2, 8, 128, 128])
    a_b_s_d = din("a_b_s", [2, 8 * 128])
    a_w_out_d = din("a_w_out", [2, D, D])
    kv_ada_w_d = din("kv_ada_w", [D, 2 * D])
    kv_ada_b_d = din("kv_ada_bT", [128, 16])
    kv_norm_g_d = din("kv_norm_gT", [128, 8])
    kv_w_d = din("kv_w", [D, 1536])
    cmp_posT_d = din("cmp_posT", [2, 64, 32])
    cmp_w1_d = din("cmp_w1", [2, 2048, 256])
    cmp_b1_d = din("cmp_b1T", [128, 4])
    cmp_w2_d = din("cmp_w2", [2, 256, 64])
    cmp_b2_d = din("cmp_b2", [2, 64])
    b_w_q_d = din("b_w_q", [2, D, 1024])
    b_w_g_d = din("b_w_g", [2, D, 48])
    b_w_out_d = din("b_w_out", [2, D, D])
    ff_w_in_d = din("ff_w_in", [4, D, DFF])
    ff_w_out_d = din("ff_w_out", [4, DFF, D])
    ident_d = din("c_ident", [128, 128])
    tri_d = din("c_tri", [128, 128])
    causal_d = din("c_causal", [128, 128])
    acausal_d = din("c_acausal", [128, 128])
    cmpb_d = din("c_cmpb", [128, 16 * 128])
    E_d = din("c_E", [128, 16 * 128])
    keep_d = din("c_keep", [128, 16 * 32])
    addc_d = din("c_addc", [128, 16 * 32])
    ovl_d = din("c_ovl", [128, 33])
    sel_d = din("c_sel", [48, 48 * 64])

    def dscr(name, shape):
        return nc.dram_tensor(name, list(shape), BF16).ap()

    s_a_w_in = dscr("s_a_w_in", [2, D, 2 * D])
    s_a_w_out = dscr("s_a_w_out", [2, D, D])
    s_kv_w = dscr("s_kv_w", [D, 1536])
    s_cmp_w1 = dscr("s_cmp_w1", [2, 2048, 256])
    s_b_w_q = dscr("s_b_w_q", [2, D, 1024])
    s_b_w_out = dscr("s_b_w_out", [2, D, D])
    s_ff_w_in = dscr("s_ff_w_in", [4, D, DFF])
    s_ff_w_out = dscr("s_ff_w_out", [4, DFF, D])
    s_wsT = dscr("s_wsT", [2, 128, 1024])
    s_bm = nc.dram_tensor("s_bm", [2, 128, 1024], F32).ap()
    scr_ws_d = [Tl(), Tl()]
    scr_bm_d = [Tl(), Tl()]

    cur = [16512]
    top = nc.sbuf_top

    def sb(name, shape, dt, at=None):
        n = 1
        for s in shape[1:]:
            n *= s
        nbytes = n * (4 if dt == F32 else 2)
        nbytes = (nbytes + 31) // 32 * 32
        if at is None:
            off = cur[0]
            cur[0] += nbytes
            assert cur[0] <= top, ("SBUF overflow", name, cur[0], top)
        else:
            off = at
        return Buf(nc.alloc_sbuf_tensor_at(name, list(shape), dt, offset=off))

    xT = [sb("xT%d" % c, [128, TT], F32) for c in range(8)]
    hT = [sb("hT%d" % c, [128, TT], BF16) for c in range(8)]
    yT = [sb("yT%d" % c, [128, TT], F32) for c in range(8)]
    sq = [sb("sq%d" % i, [128, TT], BF16) for i in range(2)]
    rstd = sb("rstd", [128, TT], F32)
    tmpf = [sb("tmpf%d" % i, [128, TT], F32) for i in range(2)]
    NSLOT = 4
    wslot = [sb("wslot%d" % i, [128, 4096], BF16) for i in range(NSLOT)]
    wsem = [P.new_sem("w") for _ in range(NSLOT)]
    xio_sem = [P.new_sem("xio") for _ in range(4)]
    k_selT = sb("k_selT", [128, 2, T], BF16)
    k_winT = sb("k_winT", [128, 2, T], BF16)
    VW = 16 * 512
    vsel_all = sb("vsel_all", [128, VW], BF16)
    vwin_all = sb("vwin_all", [128, VW], BF16)
    vsel_d = [Tl() for _ in range(16)]
    vwin_d = [Tl() for _ in range(16)]
    vones_d = Tl()
    k_cmpT = sb("k_cmpT", [128, 2, 128], BF16)
    v_cmp = sb("v_cmp", [128, 4 * 128], BF16)
    raw_k = sb("raw_k", [128, 2, 16 + TT], BF16)
    raw_v = sb("raw_v", [128, 2, 16 + TT], BF16)
    ident = sb("ident", [128, 128], F32)
    ident_bf = sb("ident_bf", [128, 128], BF16)
    ones_bf = sb("ones_bf", [128, 128], BF16)
    tri = sb("tri", [128, 128], F32)
    causal = sb("causal", [128, 128], BF16)
    acausal = sb("acausal", [128, 128], BF16)
    cmpb = sb("cmpb", [128, 16, 128], BF16)
    Ec = sb("Ec", [128, 16, 128], BF16)
    selbT = [sb("selbT%d" % g, [128, 128], BF16) for g in range(4)]
    selb_f = [sb("selb_f%d" % g, [128, 32], F32) for g in range(4)]
    keepc = sb("keepc", [128, 16, 32], BF16)
    addcc = sb("addcc", [128, 16, 32], BF16)
    ovl = sb("ovl", [128, 33], BF16)
    selc = sb("selc", [48, 48, 64], BF16)
    cst = sb("cst", [128, BPC, 4, 6, 8], F32)
    cstkv = sb("cstkv", [128, BPC, 2, 8], F32)
    wg = sb("wg", [128, 2, 8, 48], BF16)
    w2k = sb("w2k", [128, 2, 192], BF16)
    w2v = sb("w2v", [128, 2, 64], BF16)
    b2k = sb("b2k", [128, 1], F32)
    b2v = sb("b2v", [1, 64], BF16)
    posb = sb("posb", [128, 2, 2], F32)
    lnb = sb("lnb", [128, 16], F32)
    small = sb("small", [128, 64], F32)
    epsc = sb("epsc", [128, 1], F32)
    hkw = sb("hkw", [128, 2, 2, 4, 32], BF16)
    posT = sb("posT", [64, 2, 32], BF16)
    arena0 = cur[0]

    vtok = [sb("vtok%d" % i, [128, D], F32) for i in range(2)]
    vhat = [sb("vhat%d" % i, [128, D], BF16) for i in range(2)]
    uT = [sb("uT%d" % c, [128, TT], BF16) for c in range(8)]
    oTg = [sb("oTg%d" % c, [128, TT], BF16) for c in range(8)]
    gbc = sb("gbc", [128, D], F32)
    bnst = sb("bnst", [128, 32], F32)
    wsT_l = sb("wsT_l", [128, 8, 128], BF16)
    bm_l = sb("bm_l", [128, 8, 128], F32)
    arena_end = cur[0]
    tilesA = vtok + vhat + uT + oTg + [gbc, bnst, wsT_l, bm_l]
    cur[0] = arena0
    qT_all = sb("qT_all", [128, 8, TT], BF16)
    gsT = sb("gsT", [48, TT], BF16)
    pT = [sb("pT%d" % i, [128, TT], BF16) for i in range(3)]
    o_acc = [sb("o_acc%d" % i, [64, TT], F32) for i in range(4)]
    o_tmp = [sb("o_tmp%d" % i, [64, TT], F32) for i in range(1)]
    rden = [sb("rden%d" % i, [64, TT], F32) for i in range(1)]
    ffac = [sb("ffac%d" % i, [64, TT], F32) for i in range(1)]
    oT_all = sb("oT_all", [64, 16, TT], BF16)
    impb = sb("impb", [128, 4, 64], F32)
    arena_end = max(arena_end, cur[0])
    tilesB = [qT_all] + [gsT] + pT + o_acc + o_tmp + rden + ffac + [oT_all] + [impb]
    cur[0] = arena0
    xio = [sb("xio%d" % s, [128, D], F32) for s in range(4)]
    stage = [sb("stage%d" % i, [128, 2048], F32) for i in range(2)]
    stage_sem = [P.new_sem("stg") for _ in range(2)]
    modT = sb("modT", [128, 5, 48, BPC], F32)
    adab = sb("adab", [128, 5, 48], F32)
    normg = sb("normg", [128, 17, 8], F32)
    wsT_p = sb("wsT_p", [128, 8, 128], BF16)
    bm_p = sb("bm_p", [128, 8, 128], F32)
    arena_end = max(arena_end, cur[0])
    tilesC = xio + stage + [modT, adab, normg, wsT_p, bm_p]
    cur[0] = arena0
    hid = [[sb("hid%d_%d" % (i, c), [128, TT], BF16) for c in range(4)] for i in range(2)]
    relu = [sb("relu%d" % i, [128, TT], F32) for i in range(2)]
    arena_end = max(arena_end, cur[0])
    tilesD = hid[0] + hid[1] + relu
    cur[0] = arena_end
    assert cur[0] <= top, ("SBUF overflow", cur[0], top)
    print("SBUF used", cur[0], "of", top)
    overlays = {"A": tilesA, "B": tilesB, "C": tilesC, "D": tilesD}

    def phase(name):
        old = []
        for k, v in overlays.items():
            if k != name:
                old.extend(t.d for t in v)
        P.handover(old, [t.d for t in overlays[name]])

    psb = [Buf(nc.alloc_psum_tensor("ps%d" % i, [128, 512], F32)) for i in range(8)]
    psi = [0]

    PS_GEN = [0, 1, 2, 3, 7]

    def ps():
        b = psb[PS_GEN[psi[0] % 5]]
        psi[0] += 1
        return b

    acc_i = [0]
    pT_rot = [0]
    pss_i = [0]
    psm_i = [0]

    def ps_s():
        b = psb[pss_i[0] % 3]
        pss_i[0] += 1
        return b

    def ps_m():
        b = psb[(3, 7)[psm_i[0] % 2]]
        psm_i[0] += 1
        return b

    def ps_acc():
        k = acc_i[0] % 3
        acc_i[0] += 1
        return psb[4 + k]


    def vaug(buf, blk, rows=128):
        return buf.t[0:rows, blk * 128:(blk + 1) * 128]

    def mmg(out_ap, pairs, reads, wr, first=True, last=True):
        n = len(pairs)

        def fn(e):
            r = None
            for i, (l, rr) in enumerate(pairs):
                r = e.matmul(out_ap, l, rr, start=(first and i == 0), stop=(last and i == n - 1))
            return r

        P.op("pe", fn, reads=reads, writes=[wr])

    def mm_multi(items, reads, wr):
        def fn(e):
            r = None
            for (o, l, rr) in items:
                r = e.matmul(o, l, rr, start=True, stop=True)
            return r

        P.op("pe", fn, reads=reads, writes=[wr])

    def act(out_ap, in_ap, func, reads, wr, bias=None, scale=None):
        kw = {}
        if bias is not None:
            kw["bias"] = bias
        if scale is not None:
            kw["scale"] = scale
        P.op("act", lambda e: e.activation(out=out_ap, in_=in_ap, func=func, **kw), reads=reads, writes=[wr])

    def tt(eng, out_ap, a, b, op, reads, wr):
        P.op(eng, lambda e: e.tensor_tensor(out=out_ap, in0=a, in1=b, op=op), reads=reads, writes=[wr])

    def stt(eng, out_ap, a, scalar, b, op0, op1, reads, wr):
        P.op(eng, lambda e: e.scalar_tensor_tensor(out=out_ap, in0=a, scalar=scalar, in1=b, op0=op0, op1=op1),
             reads=reads, writes=[wr])

    def ts(eng, out_ap, a, s1, s2, op0, op1, reads, wr):
        if op1 is None:
            P.op(eng, lambda e: e.tensor_scalar(out=out_ap, in0=a, scalar1=s1, scalar2=None, op0=op0),
                 reads=reads, writes=[wr])
        else:
            P.op(eng, lambda e: e.tensor_scalar(out=out_ap, in0=a, scalar1=s1, scalar2=s2, op0=op0, op1=op1),
                 reads=reads, writes=[wr])

    def cp(eng, out_ap, in_ap, reads, wr):
        P.op(eng, lambda e: e.tensor_copy(out=out_ap, in_=in_ap), reads=reads, writes=[wr])

    def dma(eng, out_ap, in_ap, sem, reads, wr):
        P.op(eng, lambda e: e.dma_start(out=out_ap, in_=in_ap), reads=reads, writes=[wr], dma_sem=sem)

    def recip(out_ap, in_ap, reads, wr):
        P.op("dve", lambda e: e.reciprocal(out=out_ap, in_=in_ap), reads=reads, writes=[wr])

    def transp(out_ap, in_ap, idn, reads, wr):
        P.op("pe", lambda e: e.transpose(out_ap, in_ap, idn), reads=reads, writes=[wr])

    def memset(eng, ap, val, wr):
        P.op(eng, lambda e: e.memset(ap, val), writes=[wr])

    _bsem = {}

    def bsem(buf):
        k = id(buf)
        if k not in _bsem:
            _bsem[k] = P.new_sem("b")
        return _bsem[k]

    def ldc(eng, buf, out_ap, src):
        dma(eng, out_ap, src, bsem(buf), [], buf.d)

    def load_x(b, i):
        t0 = i * TT
        for s in range(4):
            dma("pool", xio[s].t[:, :], x_d[b, t0 + s * 128:t0 + (s + 1) * 128, :], xio_sem[s], [], xio[s].d)

    ldc("sp", ident, ident.t[:, :], ident_d)
    ldc("sp", tri, tri.t[:, :], tri_d)
    ldc("pool", keepc, keepc.t[:, :, :], keep_d.rearrange("p (a b) -> p a b", b=32))
    ldc("pool", addcc, addcc.t[:, :, :], addc_d.rearrange("p (a b) -> p a b", b=32))
    ldc("pool", ident_bf, ident_bf.t[:, :], ident_d)
    ldc("pool", causal, causal.t[:, :], causal_d)
    ldc("pool", acausal, acausal.t[:, :], acausal_d)
    ldc("pool", cmpb, cmpb.t[:, :, :], cmpb_d.rearrange("p (a b) -> p a b", b=128))
    ldc("pool", Ec, Ec.t[:, :, :], E_d.rearrange("p (a b) -> p a b", b=128))
    ldc("pool", ovl, ovl.t[:, :], ovl_d)
    ldc("pool", selc, selc.t[:, :, :], sel_d.rearrange("p (a b) -> p a b", b=64))
    memset("dve", ones_bf.t[:, :], 1.0, ones_bf.d)
    for g in range(4):
        memset("dve", selbT[g].t[:, :], 0.0, selbT[g].d)
    memset("dve", epsc.t[:, :], EPS, epsc.d)
    memset("dve", vsel_all.t[:, :], 1.0, vones_d)
    memset("dve", vwin_all.t[:, :], 1.0, vones_d)
    memset("dve", v_cmp.t[:, :], 1.0, vones_d)
    for l in range(2):
        ldc("pool", wg, wg.t[:, l, :, :], b_w_g_d[l].rearrange("(k p) n -> p k n", p=128))
    memset("dve", w2k.t[:, :, :], 0.0, w2k.d)
    ldc("pool", w2k, w2k.t[:, :, 64:128], cmp_w2_d[0].rearrange("(k p) n -> p k n", p=128))
    ldc("pool", w2v, w2v.t[:, :, :], cmp_w2_d[1].rearrange("(k p) n -> p k n", p=128))
    ldc("sp", b2k, b2k.t[0:64, :], cmp_b2_d[0].rearrange("(p o) -> p o", o=1))
    ldc("sp", b2k, b2k.t[64:128, :], cmp_b2_d[0].rearrange("(p o) -> p o", o=1))
    ldc("pool", b2v, b2v.t[:, :], cmp_b2_d[1].rearrange("(o n) -> o n", o=1))
    ldc("sp", lnb, lnb.t[:, :], a_ln_b_d)
    ldc("pool", posT, posT.t[:, :, :], cmp_posT_d.rearrange("k d l -> d k l"))
    ldc("sp", posb, posb.t[:, :, :], cmp_b1_d.rearrange("p (k j) -> p k j", j=2))
    ldc("sp", small, small.t[:, 0:16], cT_d.rearrange("p k b -> p (k b)"))
    ldc("sp", adab, adab.t[:, 0:4, :], ada_b_d.rearrange("p (l c) -> p l c", c=48))
    ldc("sp", adab, adab.t[:, 4, 0:16], kv_ada_b_d)
    ldc("sp", normg, normg.t[:, 0:16, :], norm_g_d.rearrange("p (l c) -> p l c", c=8))
    ldc("sp", normg, normg.t[:, 16, :], kv_norm_g_d)

    load_x(0, 0)
    conv = {}

    def convert(key, src, dst, nelem):
        sem = P.new_sem("cv")
        tl = Tl()
        cols = nelem // 128
        s2 = src.rearrange("(p n) -> p n", p=128) if False else src
        step = 8192
        for c0 in range(0, cols, step):
            c1 = min(cols, c0 + step)
            dma("pool", dst[:, c0:c1], src[:, c0:c1], sem, [], tl)
        conv[key] = tl

    def flat2(ap, n):
        nd = len(ap.shape)
        names = " ".join("d%d" % i for i in range(nd))
        flat = ap.rearrange("%s -> (%s)" % (names, names))
        return flat.rearrange("(p n) -> p n", p=128)

    def conv_w(key, src_ap, dst_ap):
        n = 1
        for s in src_ap.shape:
            n *= s
        convert(key, flat2(src_ap, n), flat2(dst_ap, n), n)

    for l in range(2):
        conv_w(("a_in", l), a_w_in_d[l], s_a_w_in[l])
        conv_w(("a_out", l), a_w_out_d[l], s_a_w_out[l])
        conv_w(("ff_in", l), ff_w_in_d[l], s_ff_w_in[l])
        conv_w(("ff_out", l), ff_w_out_d[l], s_ff_w_out[l])
    conv_w(("kv",), kv_w_d, s_kv_w)
    conv_w(("w1",), cmp_w1_d, s_cmp_w1)
    for l in range(2, 4):
        conv_w(("b_q", l), b_w_q_d[l - 2], s_b_w_q[l - 2])
        conv_w(("b_out", l), b_w_out_d[l - 2], s_b_w_out[l - 2])
        conv_w(("ff_in", l), ff_w_in_d[l], s_ff_w_in[l])
        conv_w(("ff_out", l), ff_w_out_d[l], s_ff_w_out[l])

    act(small.t[:, 16:32], small.t[:, 0:16], AF.Silu, [small.d], small.d)
    cactv = small.t[:, 16:32].rearrange("p (k b) -> p k b", b=BPC)
    bi = 0
    phase("C")
    for l in range(5):
        nblk = 24 if l < 4 else 8
        src = (ada_w_d[l] if l < 4 else kv_ada_w_d)
        for blk in range(nblk):
            st = stage[bi % 2]
            ssem = stage_sem[bi % 2]
            bi += 1
            stv = st.t[:, :].rearrange("p (k n) -> p k n", n=256)
            dma("sp", stv, src[:, blk * 256:(blk + 1) * 256].rearrange("(k p) n -> p k n", p=128), ssem, [], st.d)
            for cc in range(2):
                pb = ps()
                mmg(pb.t[:, 0:BPC], [(stv[:, k, cc * 128:(cc + 1) * 128], cactv[:, k, :]) for k in range(8)],
                    [st.d, small.d], pb.d)
                ch = blk * 2 + cc
                ts("dve", modT.t[:, l, ch, :], pb.t[:, 0:BPC], adab.t[:, l, ch:ch + 1], None, ALU.add, None,
                   [pb.d, adab.d], modT.d)
    for b in range(BPC):
        for l in range(4):
            for (kind, sc_off, gi) in ((0, 8, 0), (3, 32, 2)):
                stt("dve", cst.t[:, b, l, kind, :], modT.t[:, l, sc_off:sc_off + 8, b], 1.0, normg.t[:, l * 4 + gi, :],
                    ALU.add, ALU.mult, [modT.d, normg.d], cst.d)
            for (kind, sh_off) in ((1, 0), (4, 24)):
                cp("dve", cst.t[:, b, l, kind, :], modT.t[:, l, sh_off:sh_off + 8, b], [modT.d], cst.d)
            for (kind, g_off, gi) in ((2, 16, 1), (5, 40, 3)):
                tt("dve", cst.t[:, b, l, kind, :], modT.t[:, l, g_off:g_off + 8, b], normg.t[:, l * 4 + gi, :], ALU.mult,
                   [modT.d, normg.d], cst.d)
        stt("dve", cstkv.t[:, b, 0, :], modT.t[:, 4, 8:16, b], 1.0, normg.t[:, 16, :], ALU.add, ALU.mult,
            [modT.d, normg.d], cstkv.d)
        cp("dve", cstkv.t[:, b, 1, :], modT.t[:, 4, 0:8, b], [modT.d], cstkv.d)

    for l in range(2):
        st = stage[bi % 2]
        ssem = stage_sem[bi % 2]
        bi += 1
        wv = st.t[:, 0:1024].rearrange("p (g s) -> p g s", s=128)
        dma("sp", wv, a_w_s_d[l].rearrange("g t s -> t g s"), ssem, [], st.d)
        tt("dve", wv, wv, tri.t[:, :].unsqueeze(1).to_broadcast([128, 8, 128]), ALU.mult, [st.d, tri.d], st.d)
        for half in range(2):
            pb = ps()
            for gg in range(4):
                g = half * 4 + gg
                transp(pb.t[:, gg * 128:(gg + 1) * 128], wv[:, g, :], ident.t[:, :], [st.d, ident.d], pb.d)
            cp("dve", wsT_p.t[:, half * 4:(half + 1) * 4, :], pb.t[:, :].rearrange("p (g t) -> p g t", t=128), [pb.d], wsT_p.d)
        ldc("sp", bm_p, bm_p.t[:, :, :], a_b_s_d[l].partition_broadcast(128).rearrange("p (g t) -> p g t", t=128))
        for half in range(2):
            pb = ps()
            mmg(pb.t[:, :], [(ones_bf.t[:, :], wsT_p.t[:, half * 4:(half + 1) * 4, :])], [ones_bf.d, wsT_p.d], pb.d)
            for gg in range(4):
                g = half * 4 + gg
                stt("dve", bm_p.t[:, g, :], pb.t[:, gg * 128:(gg + 1) * 128], lnb.t[:, l * 8 + g:l * 8 + g + 1],
                    bm_p.t[:, g, :], ALU.mult, ALU.add, [pb.d, lnb.d, bm_p.d], bm_p.d)
        dma("sp", s_wsT[l], wsT_p.t[:, :, :].rearrange("p g t -> p (g t)"), bsem(wsT_p), [wsT_p.d], scr_ws_d[l])
        dma("sp", s_bm[l], bm_p.t[:, :, :].rearrange("p g t -> p (g t)"), bsem(bm_p), [bm_p.d], scr_bm_d[l])
        wsT_p.d.addr(scr_ws_d[l].w)
        bm_p.d.addr(scr_bm_d[l].w)

    w1tl = conv[("w1",)]
    for kv in range(2):
        for jc in range(2):
            slot = wslot[(kv * 2 + jc) % NSLOT]
            sem = wsem[(kv * 2 + jc) % NSLOT]
            sv = slot.t[0:64, :].rearrange("p (l j) -> p l j", j=128)
            dma("sp", sv, s_cmp_w1[kv].rearrange("(l d) j -> d l j", d=64)[:, :, jc * 128:(jc + 1) * 128], sem, [w1tl], slot.d)
            pb = ps()
            mmg(pb.t[:, 0:1], [(sv[:, l, :], posT.t[:, kv, l:l + 1]) for l in range(32)], [slot.d, posT.d], pb.d)
            tt("dve", posb.t[:, kv, jc:jc + 1], pb.t[:, 0:1], posb.t[:, kv, jc:jc + 1], ALU.add, [pb.d, posb.d], posb.d)

    def wload(slot_i, out_ap, src_ap, dep):
        dma("sp", out_ap, src_ap, wsem[slot_i], [dep], wslot[slot_i].d)

    slot_rr = [0]

    def next_slot():
        i = slot_rr[0] % NSLOT
        slot_rr[0] += 1
        return i

    def rms_stats(src):
        pb = ps()
        for c in range(8):
            s = sq[c % 2]
            act(s.t[:, :], src[c].t[:, :], AF.Square, [src[c].d], s.d)
            mmg(pb.t[:, :], [(ones_bf.t[:, :], s.t[:, :])], [ones_bf.d, s.d], pb.d, first=(c == 0), last=(c == 7))
        act(rstd.t[:, :], pb.t[:, :], AF.Ln, [pb.d, epsc.d], rstd.d, bias=epsc.t[:, 0:1], scale=1.0 / D)
        act(rstd.t[:, :], rstd.t[:, :], AF.Exp, [rstd.d], rstd.d, scale=-0.5)

    def rmsmod(Gap, Sap):
        rms_stats(xT)
        for c in range(8):
            tf = tmpf[c % 2]
            stt("dve", tf.t[:, :], xT[c].t[:, :], Gap[:, c:c + 1], rstd.t[:, :], ALU.mult, ALU.mult,
                [xT[c].d, rstd.d, cst.d, cstkv.d], tf.d)
            act(hT[c].t[:, :], tf.t[:, :], AF.Identity, [tf.d, cst.d, cstkv.d], hT[c].d, bias=Sap[:, c:c + 1], scale=1.0)

    def residual(GGap):
        rms_stats(yT)
        for c in range(8):
            tf = tmpf[c % 2]
            stt("dve", tf.t[:, :], yT[c].t[:, :], GGap[:, c:c + 1], rstd.t[:, :], ALU.mult, ALU.mult,
                [yT[c].d, rstd.d, cst.d], tf.d)
            tt("pool", xT[c].t[:, :], xT[c].t[:, :], tf.t[:, :], ALU.add, [xT[c].d, tf.d], xT[c].d)

    def ffn(b, l):
        phase("D")
        rmsmod(cst.t[:, b, l, 3, :], cst.t[:, b, l, 4, :])
        tin = conv[("ff_in", l)]
        tout = conv[("ff_out", l)]
        hds = [h.d for h in hT]
        slots = {}

        def load(j):
            sa = next_slot()
            sbi = next_slot()
            A = wslot[sa].t[:, :].rearrange("p (k n) -> p k n", n=512)
            B = wslot[sbi].t[:, :].rearrange("p (k n) -> p k n", n=1024)
            wload(sa, A, s_ff_w_in[l][:, j * 512:(j + 1) * 512].rearrange("(k p) n -> p k n", p=128), tin)
            wload(sbi, B, s_ff_w_out[l][j * 512:(j + 1) * 512, :].rearrange("(k p) n -> p k n", p=128), tout)
            slots[j] = (sa, sbi, A, B)

        def hidden(j):
            sa, sbi, A, B = slots[j]
            hb = hid[j % 2]
            for c in range(4):
                pb = ps()
                mmg(pb.t[:, :], [(A[:, k, c * 128:(c + 1) * 128], hT[k].t[:, :]) for k in range(8)],
                    [wslot[sa].d] + hds, pb.d)
                r = relu[c % 2]
                act(r.t[:, :], pb.t[:, :], AF.Relu, [pb.d], r.d)
                tt("pool", hb[c].t[:, :], r.t[:, :], r.t[:, :], ALU.mult, [r.d], hb[c].d)

        def ypart(j):
            sa, sbi, A, B = slots[j]
            hb = hid[j % 2]
            for n in range(8):
                pb = ps()
                mmg(pb.t[:, :], [(B[:, c, n * 128:(n + 1) * 128], hb[c].t[:, :]) for c in range(4)],
                    [wslot[sbi].d] + [h.d for h in hb], pb.d)
                if j == 0:
                    act(yT[n].t[:, :], pb.t[:, :], AF.Copy, [pb.d], yT[n].d)
                else:
                    tt("dve", yT[n].t[:, :], pb.t[:, :], yT[n].t[:, :], ALU.add, [pb.d, yT[n].d], yT[n].d)

        load(0)
        hidden(0)
        for j in range(8):
            if j + 1 < 8:
                load(j + 1)
                hidden(j + 1)
            ypart(j)
        residual(cst.t[:, b, l, 5, :])

    def gmlp(b, l):
        phase("A")
        rmsmod(cst.t[:, b, l, 0, :], cst.t[:, b, l, 1, :])
        tin = conv[("a_in", l)]
        hds = [h.d for h in hT]
        ldc("sp", gbc, gbc.t[:, :], a_ln_g_d[l].partition_broadcast(128))
        dma("sp", wsT_l.t[:, :, :].rearrange("p g t -> p (g t)"), s_wsT[l], bsem(wsT_l), [scr_ws_d[l]], wsT_l.d)
        dma("sp", bm_l.t[:, :, :].rearrange("p g t -> p (g t)"), s_bm[l], bsem(bm_l), [scr_bm_d[l]], bm_l.d)
        for half in range(2):
            si = next_slot()
            W = wslot[si].t[:, :].rearrange("p (k n) -> p k n", n=512)
            wload(si, W, s_a_w_in[l][:, half * 512:(half + 1) * 512].rearrange("(k p) n -> p k n", p=128), tin)
            for cc in range(4):
                n = half * 4 + cc
                pb = ps()
                mmg(pb.t[:, :], [(W[:, k, cc * 128:(cc + 1) * 128], hT[k].t[:, :]) for k in range(8)],
                    [wslot[si].d] + hds, pb.d)
                act(uT[n].t[:, :], pb.t[:, :], AF.Gelu_apprx_tanh, [pb.d], uT[n].d)
        sv = [next_slot(), next_slot()]
        Wv = []
        for half in range(2):
            W = wslot[sv[half]].t[:, :].rearrange("p (k n) -> p k n", n=512)
            wload(sv[half], W, s_a_w_in[l][:, D + half * 512:D + (half + 1) * 512].rearrange("(k p) n -> p k n", p=128), tin)
            Wv.append(W)
        def vmat(s):
            vt = vtok[s % 2]
            for half in range(2):
                pb = ps()
                mmg(pb.t[:, :], [(hT[k].t[:, s * 128:(s + 1) * 128], Wv[half][:, k, :]) for k in range(8)],
                    [wslot[sv[half]].d] + hds, pb.d)
                act(vt.t[:, half * 512:(half + 1) * 512], pb.t[:, :], AF.Gelu_apprx_tanh, [pb.d], vt.d)

        def vrest(s):
            vt = vtok[s % 2]
            vh = vhat[s % 2]
            for half in range(2):
                P.op("dve", lambda e, o=bnst.t[:, half * 6:half * 6 + 6], i=vt.t[:, half * 512:(half + 1) * 512]: e.bn_stats(out=o, in_=i),
                     reads=[vt.d], writes=[bnst.d])
            P.op("dve", lambda e, o=bnst.t[:, 16:18], i=bnst.t[:, 0:12].rearrange("p (a b) -> p a b", b=6): e.bn_aggr(out=o, in_=i),
                 reads=[bnst.d], writes=[bnst.d])
            act(bnst.t[:, 18:19], bnst.t[:, 17:18], AF.Sqrt, [bnst.d, epsc.d], bnst.d, bias=epsc.t[:, 0:1], scale=1.0)
            recip(bnst.t[:, 19:20], bnst.t[:, 18:19], [bnst.d], bnst.d)
            stt("dve", vt.t[:, :], vt.t[:, :], bnst.t[:, 16:17], gbc.t[:, :], ALU.subtract, ALU.mult,
                [vt.d, bnst.d, gbc.d], vt.d)
            act(vh.t[:, :], vt.t[:, :], AF.Identity, [vt.d, bnst.d], vh.d, scale=bnst.t[:, 19:20])
            for half in range(2):
                pb = ps()
                mm_multi([(pb.t[:, gg * 128:(gg + 1) * 128], vh.t[:, (half * 4 + gg) * 128:(half * 4 + gg + 1) * 128],
                           wsT_l.t[:, half * 4 + gg, :]) for gg in range(4)], [vh.d, wsT_l.d], pb.d)
                tf = tmpf[half]
                tt("dve", tf.t[:, :], pb.t[:, :], bm_l.t[:, half * 4:(half + 1) * 4, :].rearrange("p g t -> p (g t)"),
                   ALU.add, [pb.d, bm_l.d], tf.d)
                for gg in range(4):
                    g = half * 4 + gg
                    tt("pool", oTg[g].t[:, s * 128:(s + 1) * 128], tf.t[:, gg * 128:(gg + 1) * 128],
                       uT[g].t[:, s * 128:(s + 1) * 128], ALU.mult, [tf.d, uT[g].d], oTg[g].d)

        vmat(0)
        for s in range(4):
            if s + 1 < 4:
                vmat(s + 1)
            vrest(s)
        tout = conv[("a_out", l)]
        ods = [o.d for o in oTg]
        for half in range(2):
            si = next_slot()
            W = wslot[si].t[:, :].rearrange("p (k n) -> p k n", n=512)
            wload(si, W, s_a_w_out[l][:, half * 512:(half + 1) * 512].rearrange("(k p) n -> p k n", p=128), tout)
            for cc in range(4):
                n = half * 4 + cc
                pb = ps()
                mmg(pb.t[:, :], [(W[:, k, cc * 128:(cc + 1) * 128], oTg[k].t[:, :]) for k in range(8)],
                    [wslot[si].d] + ods, pb.d)
                act(yT[n].t[:, :], pb.t[:, :], AF.Copy, [pb.d], yT[n].d)
        residual(cst.t[:, b, l, 2, :])

    def kvproj(b, i):
        rmsmod(cstkv.t[:, b, 0, :], cstkv.t[:, b, 1, :])
        tkv = conv[("kv",)]
        hds = [h.d for h in hT]
        t0 = i * TT
        for blk in range(3):
            si = next_slot()
            W = wslot[si].t[:, :].rearrange("p (k n) -> p k n", n=512)
            wload(si, W, s_kv_w[:, blk * 512:(blk + 1) * 512].rearrange("(k p) n -> p k n", p=128), tkv)
            for gh in range(2):
                pb = ps()
                mmg(pb.t[:, :], [(W[:, k, gh * 128:(gh + 1) * 128], hT[k].t[:, :]) for k in range(8)], [wslot[si].d] + hds, pb.d)
                if blk == 0:
                    act(raw_k.t[:, gh, 16:16 + TT], pb.t[:, :], AF.Copy, [pb.d], raw_k.d)
                elif blk == 1:
                    act(k_selT.t[:, gh, t0:t0 + TT], pb.t[:, :], AF.Copy, [pb.d], k_selT.d)
                else:
                    act(k_winT.t[:, gh, t0:t0 + TT], pb.t[:, :], AF.Copy, [pb.d], k_winT.d)
            if blk == 0:
                for gh in range(2):
                    pb = ps()
                    mmg(pb.t[:, :], [(W[:, k, 256 + gh * 128:256 + (gh + 1) * 128], hT[k].t[:, :]) for k in range(8)],
                        [wslot[si].d] + hds, pb.d)
                    act(raw_v.t[:, gh, 16:16 + TT], pb.t[:, :], AF.Copy, [pb.d], raw_v.d)
            else:
                vv, vd = (vsel_all, vsel_d) if blk == 1 else (vwin_all, vwin_d)
                for s in range(4):
                    kt = i * 4 + s
                    pb = ps()
                    mmg(pb.t[:, 0:256], [(hT[k].t[:, s * 128:(s + 1) * 128], W[:, k, 256:512]) for k in range(8)],
                        [wslot[si].d] + hds, pb.d)
                    cp("dve", vv.t[:, kt * 512:(kt + 1) * 512].rearrange("p (g c) -> p g c", c=128)[:, :, 0:64],
                       pb.t[:, 0:256].rearrange("p (g d) -> p g d", d=64), [pb.d, vones_d], vd[kt])
        for kv in range(2):
            raw = raw_k if kv == 0 else raw_v
            for jc in range(2):
                si = next_slot()
                sv = wslot[si].t[:, :].rearrange("p (l j) -> p l j", j=128)
                src = s_cmp_w1[kv].rearrange("(l d) j -> d l j", d=64)[:, :, jc * 128:(jc + 1) * 128]
                wload(si, sv[0:64], src, conv[("w1",)])
                wload(si, sv[64:128], src, conv[("w1",)])
                for ph in range(2):
                    pb = ps()
                    mmg(pb.t[:, 0:64].rearrange("p (a m) -> p a m", m=32),
                        [(sv[ph * 64:(ph + 1) * 64, l, :], raw.t[ph * 64:(ph + 1) * 64, :, l:l + 16 * 31 + 1:16]) for l in range(32)],
                        [wslot[si].d, raw.d], pb.d)
                    for gh in range(2):
                        act(hkw.t[:, jc, kv, gh * 2 + ph, :], pb.t[:, gh * 32:(gh + 1) * 32], AF.Gelu_apprx_tanh,
                            [pb.d, posb.d], hkw.d, bias=posb.t[:, kv, jc:jc + 1], scale=1.0)
        for gh in range(2):
            pb = ps()
            pairs = []
            for ph in range(2):
                for jc in range(2):
                    lw = w2k.t[:, jc, 64:192] if ph == 0 else w2k.t[:, jc, 0:128]
                    pairs.append((lw, hkw.t[:, jc, 0, gh * 2 + ph, :]))
            mmg(pb.t[:, 0:32], pairs, [w2k.d, hkw.d], pb.d)
            act(k_cmpT.t[:, gh, i * 32:(i + 1) * 32], pb.t[:, 0:32], AF.Identity, [pb.d, b2k.d], k_cmpT.d,
                bias=b2k.t[:, 0:1], scale=1.0)
        pb = ps()
        for g in range(4):
            pairs = [(hkw.t[:, jc, 1, g, :], w2v.t[:, jc, :]) for jc in range(2)]
            pairs.append((ones_bf.t[0:1, 0:32], b2v.t[0:1, :]))
            mmg(pb.t[0:32, g * 64:(g + 1) * 64], pairs, [hkw.d, w2v.d, b2v.d, ones_bf.d], pb.d)
        cp("dve", v_cmp.t[i * 32:(i + 1) * 32, :].rearrange("p (g c) -> p g c", c=128)[:, :, 0:64],
           pb.t[0:32, 0:256].rearrange("p (g d) -> p g d", d=64), [pb.d, vones_d], v_cmp.d)
        cp("pool", raw_k.t[:, :, 0:16], raw_k.t[:, :, TT:TT + 16], [raw_k.d], raw_k.d)
        cp("pool", raw_v.t[:, :, 0:16], raw_v.t[:, :, TT:TT + 16], [raw_v.d], raw_v.d)

    def branch_finish(psa, g, br, s, oa, first, guard, final=False):
        rd = rden[0]
        ff = ffac[0]
        ot = o_tmp[0]
        if guard:
            ts("dve", rd.t[:, :], psa.t[64:128, :], 1e-30, None, ALU.max, None, [psa.d], rd.d)
            act(rd.t[:, :], rd.t[:, :], AF.Ln, [rd.d], rd.d)
        else:
            act(rd.t[:, :], psa.t[64:128, :], AF.Ln, [psa.d], rd.d)
        act(rd.t[:, :], rd.t[:, :], AF.Exp, [rd.d], rd.d, scale=-1.0)
        pg = ps_m()
        mm_multi([(pg.t[0:64, hh * 128:(hh + 1) * 128], selc.t[:, (g * 4 + hh) * 3 + br, :], gsT.t[:, s * 128:(s + 1) * 128])
                  for hh in range(4)], [selc.d, gsT.d], pg.d)
        tt("dve", ff.t[:, :], pg.t[0:64, :], rd.t[:, :], ALU.mult, [pg.d, rd.d], ff.d)
        if first:
            tt("dve", oa.t[:, :], psa.t[0:64, :], ff.t[:, :], ALU.mult, [psa.d, ff.d], oa.d)
        else:
            tt("dve", ot.t[:, :], psa.t[0:64, :], ff.t[:, :], ALU.mult, [psa.d, ff.d], ot.d)
            if final:
                tt("pool", oT_all.t[:, g * 4:(g + 1) * 4, s * 128:(s + 1) * 128], oa.t[:, :].rearrange("p (h t) -> p h t", t=128),
                   ot.t[:, :].rearrange("p (h t) -> p h t", t=128), ALU.add, [oa.d, ot.d], oT_all.d)
            else:
                tt("pool", oa.t[:, :], oa.t[:, :], ot.t[:, :], ALU.add, [oa.d, ot.d], oa.d)

    def nsa(b, l, i):
        phase("B")
        j = l - 2
        rmsmod(cst.t[:, b, l, 0, :], cst.t[:, b, l, 1, :])
        hds = [h.d for h in hT]
        tq = conv[("b_q", l)]
        for half in range(2):
            si = next_slot()
            W = wslot[si].t[:, :].rearrange("p (k n) -> p k n", n=512)
            wload(si, W, s_b_w_q[j][:, half * 512:(half + 1) * 512].rearrange("(k p) n -> p k n", p=128), tq)
            for cc in range(4):
                n = half * 4 + cc
                pb = ps()
                mmg(pb.t[:, :], [(W[:, k, cc * 128:(cc + 1) * 128], hT[k].t[:, :]) for k in range(8)], [wslot[si].d] + hds, pb.d)
                act(qT_all.t[:, n, :], pb.t[:, :], AF.Identity, [pb.d], qT_all.d, scale=0.125)
        pb = ps()
        mmg(pb.t[0:48, :], [(wg.t[:, j, k, :], hT[k].t[:, :]) for k in range(8)], [wg.d] + hds, pb.d)
        act(gsT.t[:, :], pb.t[0:48, :], AF.Sigmoid, [pb.d], gsT.d)
        ncc = 32 * (i + 1)
        b4 = lambda ap: ap.unsqueeze(1).to_broadcast([ap.shape[0], 4, 128])

        def make_scores(pb, rows, lhsT, rhs4, extra):
            n = len(extra)
            full = pb.t[0:rows, :].rearrange("p (a t) -> p a t", t=128)
            ex = list(extra)

            def fn(e):
                r = e.matmul(full, lhsT, rhs4, start=True, stop=(n == 0), skip_group_check=True)
                for ei, (el, er) in enumerate(ex):
                    r = e.matmul(full, el, er, start=False, stop=(ei == n - 1), skip_group_check=True)
                return r
            return fn

        units = []
        for s in range(4):
            qt = i * 4 + s
            use_sel = qt >= 8
            kt0 = max(0, qt - 4)
            for g in range(4):
                units.append(("cmp", s, g, 0, True, True))
                for kt in range(kt0, qt + 1):
                    units.append(("win", s, g, kt, kt == kt0, kt == qt))
            for g in range(4):
                for kt in range(qt + 1):
                    units.append(("sel", s, g, kt, kt == 0, kt == qt))
        state = {}
        accs = {}
        deferred = []

        def emit_scores(u):
            kind, s, g, kt, ufirst, ulast = u
            qt = i * 4 + s
            gh, ph = g // 2, g % 2
            pr = slice(ph * 64, (ph + 1) * 64)
            qds = [qT_all.d]
            rhss = qT_all.t[pr, gh * 4:(gh + 1) * 4, s * 128:(s + 1) * 128]
            pb = ps_s()
            if kind == "cmp":
                P.op("pe", make_scores(pb, ncc, k_cmpT.t[pr, gh, 0:ncc], rhss, []),
                     reads=qds + [k_cmpT.d], writes=[pb.d])
            elif kind == "sel":
                extra = []
                rd = list(qds) + [k_selT.d]
                if qt >= 8:
                    extra.append((Ec.t[:, kt, :], b4(selbT[g].t[:, :])))
                    rd += [Ec.d, selbT[g].d]
                P.op("pe", make_scores(pb, 128, k_selT.t[pr, gh, kt * 128:(kt + 1) * 128], rhss, extra), reads=rd, writes=[pb.d])
            else:
                extra = []
                rd = list(qds) + [k_winT.d]
                P.op("pe", make_scores(pb, 128, k_winT.t[pr, gh, kt * 128:(kt + 1) * 128], rhss, extra), reads=rd, writes=[pb.d])
            state[u] = pb

        def emit_post(u):
            kind, s, g, kt, ufirst, ulast = u
            qt = i * 4 + s
            pb = state.pop(u)
            oa = o_acc[g]
            rows = ncc if kind == "cmp" else 128
            p = pT[psi[0] % 3]
            psi[0] += 0
            pT_rot[0] += 1
            p = pT[pT_rot[0] % 3]
            act(p.t[0:rows, :], pb.t[0:rows, :], AF.Exp, [pb.d], p.d)
            m01 = None
            if kind == "cmp":
                m01 = (cmpb.t[0:rows, qt, :], cmpb.d)
            elif kt == qt:
                m01 = (causal.t[:, :], causal.d)
            elif kind == "win" and kt == qt - 4:
                m01 = (acausal.t[:, :], acausal.d)
            if m01 is not None:
                pv3 = p.t[0:rows, :].rearrange("p (h t) -> p h t", t=128)
                tt("pool", pv3, pv3, m01[0].unsqueeze(1).to_broadcast([rows, 4, 128]), ALU.mult, [p.d, m01[1]], p.d)
            key = (kind, s, g)
            if ufirst:
                accs[key] = ps_acc()
            psa = accs[key]
            if kind == "cmp":
                lw, lwd = vaug(v_cmp, g, rows), [v_cmp.d, vones_d]
            elif kind == "sel":
                lw, lwd = vaug(vsel_all, kt * 4 + g), [vsel_d[kt], vones_d]
            else:
                lw, lwd = vaug(vwin_all, kt * 4 + g), [vwin_d[kt], vones_d]
            mmg(psa.t[:, :], [(lw, p.t[0:rows, :])], lwd + [p.d], psa.d, first=ufirst, last=ulast)
            if not ulast:
                return
            del accs[key]
            if kind == "cmp":
                use_sel = qt >= 8
                if use_sel:
                    pim = ps_m()
                    mm_multi([(pim.t[:, hh * 64:hh * 64 + 33], p.t[0:ncc, hh * 128:(hh + 1) * 128], ovl.t[0:ncc, :]) for hh in range(4)],
                             [p.d, ovl.d], pim.d)
                branch_finish(psa, g, 0, s, oa, True, qt == 0)
                if use_sel:
                    pv = pim.t[:, 0:256].rearrange("p (h c) -> p h c", c=64)
                    recip(impb.t[:, 0, 32:36], pv[:, :, 32], [pim.d], impb.d)
                    ts("dve", impb.t[:, 1, 0:32], pv[:, 0, 0:32], impb.t[:, 0, 32:33], None, ALU.mult, None, [pim.d, impb.d], impb.d)
                    for hh in range(1, 4):
                        stt("dve", impb.t[:, 1, 0:32], pv[:, hh, 0:32], impb.t[:, 0, 32 + hh:33 + hh], impb.t[:, 1, 0:32],
                            ALU.mult, ALU.add, [pim.d, impb.d], impb.d)
                    tt("dve", impb.t[:, 1, 0:32], impb.t[:, 1, 0:32], keepc.t[:, qt, :], ALU.mult, [impb.d, keepc.d], impb.d)
                    tt("dve", impb.t[:, 1, 0:32], impb.t[:, 1, 0:32], addcc.t[:, qt, :], ALU.add, [impb.d, addcc.d], impb.d)
                    P.op("dve", lambda e: e.max(out=impb.t[:, 2, 0:8], in_=impb.t[:, 1, 0:32]), reads=[impb.d], writes=[impb.d])
                    P.op("dve", lambda e: e.match_replace(out=impb.t[:, 3, 0:32], in_to_replace=impb.t[:, 2, 0:8],
                                                          in_values=impb.t[:, 1, 0:32], imm_value=-2.0), reads=[impb.d], writes=[impb.d])
                    P.op("dve", lambda e: e.max(out=impb.t[:, 2, 8:16], in_=impb.t[:, 3, 0:32]), reads=[impb.d], writes=[impb.d])
                    sb_src = selb_f[g]
                    ts("dve", sb_src.t[:, :], impb.t[:, 1, 0:32], impb.t[:, 2, 15:16], NEG, ALU.is_lt, ALU.mult, [impb.d], sb_src.d)

                    def fin(g=g, sb_src=sb_src):
                        ptr = ps_m()
                        transp(ptr.t[0:32, 0:128], sb_src.t[:, :], ident.t[:, :], [sb_src.d, ident.d], ptr.d)
                        cp("dve", selbT[g].t[0:32, :], ptr.t[0:32, 0:128], [ptr.d], selbT[g].d)
                    deferred.append([4, (s, g), fin])
            elif kind == "win":
                branch_finish(psa, g, 2, s, oa, False, False)
            else:
                branch_finish(psa, g, 1, s, oa, False, False, final=True)

        def run_deferred(force_key=None):
            keep = []
            for item in deferred:
                if item[0] <= 0 or (force_key is not None and item[1] == force_key):
                    item[2]()
                else:
                    keep.append(item)
            deferred[:] = keep

        def scores_checked(u):
            if u[0] == "sel" and u[3] == 0:
                run_deferred(force_key=(u[1], u[2]))
            emit_scores(u)

        LOOK = 2
        for idx in range(min(LOOK, len(units))):
            scores_checked(units[idx])
        for idx in range(len(units)):
            if idx + LOOK < len(units):
                scores_checked(units[idx + LOOK])
            for item in deferred:
                item[0] -= 1
            run_deferred()
            emit_post(units[idx])
        run_deferred()
        for item in list(deferred):
            item[2]()
        deferred[:] = []
        tout = conv[("b_out", l)]
        ods = [oT_all.d]
        for q4 in range(4):
            si = next_slot()
            W = wslot[si].t[0:64, :].rearrange("p (h n) -> p h n", n=256)
            wload(si, W, s_b_w_out[j][:, q4 * 256:(q4 + 1) * 256].rearrange("(h d) n -> d h n", d=64), tout)
            for cc in range(2):
                n = q4 * 2 + cc
                pb = ps()
                mmg(pb.t[:, :], [(W[:, h, cc * 128:(cc + 1) * 128], oT_all.t[:, h, :]) for h in range(16)], [wslot[si].d] + ods, pb.d)
                act(yT[n].t[:, :], pb.t[:, :], AF.Copy, [pb.d], yT[n].d)
        residual(cst.t[:, b, l, 2, :])

    nst = [0]
    for b in range(BPC):
        memset("pool", raw_k.t[:, :, 0:16], 0.0, raw_k.d)
        memset("pool", raw_v.t[:, :, 0:16], 0.0, raw_v.d)
        for i in range(NTILE):
            t0 = i * TT
            phase("C")
            if not (b == 0 and i == 0):
                load_x(b, i)
            for c in range(8):
                pb = ps()
                for s in range(4):
                    transp(pb.t[:, s * 128:(s + 1) * 128], xio[s].t[:, c * 128:(c + 1) * 128], ident.t[:, :], [xio[s].d, ident.d], pb.d)
                act(xT[c].t[:, :], pb.t[:, :], AF.Copy, [pb.d], xT[c].d)
            for l in range(min(2, n_layers)):
                gmlp(b, l)
                ffn(b, l)
            if n_layers > 2:
                kvproj(b, i)
                for l in range(2, n_layers):
                    nsa(b, l, i)
                    ffn(b, l)
            phase("C")
            for s in range(4):
                for half in range(2):
                    pb = ps()
                    for cc in range(4):
                        c = half * 4 + cc
                        transp(pb.t[:, cc * 128:(cc + 1) * 128], xT[c].t[:, s * 128:(s + 1) * 128], ident.t[:, :], [xT[c].d, ident.d], pb.d)
                    act(xio[s].t[:, half * 512:(half + 1) * 512], pb.t[:, :], AF.Copy, [pb.d], xio[s].d)
                dma("pool", out_d[b, t0 + s * 128:t0 + (s + 1) * 128, :], xio[s].t[:, :], xio_sem[s], [xio[s].d], xio[s].d)
                nst[0] += 1
    finals = []
    for s in range(4):
        finals.append((xio_sem[s], P.dcnt[id(xio_sem[s])]))
    print("ops:", {e: len(v) for e, v in P.ops.items()})
    P.emit(finals)
    return nc


def _consts():
    c = {}
    c["c_ident"] = np.eye(128, dtype=np.float32)
    tl = np.arange(128)
    c["c_tri"] = (tl[None, :] <= tl[:, None]).astype(np.float32)
    c["c_causal"] = (tl[:, None] <= tl[None, :]).astype(np.float32)
    c["c_acausal"] = (tl[:, None] > tl[None, :]).astype(np.float32)
    cc = np.arange(128)[:, None, None]
    qt = np.arange(16)[None, :, None]
    t = qt * 128 + tl[None, None, :]
    valid = (cc >= 1) & (16 * (cc - 1) + 31 <= t)
    c["c_cmpb"] = valid.astype(np.float32).reshape(128, 16 * 128)
    j = np.arange(32)[:, None, None]
    key = np.arange(16)[None, :, None] * 128 + tl[None, None, :]
    Ef = np.zeros((128, 16 * 128), np.float32)
    Ef[0:32] = ((key // 64) == j).astype(np.float32).reshape(32, 16 * 128)
    c["c_E"] = Ef
    tq = np.arange(16)[None, :, None] * 128 + tl[:, None, None]
    curb = tq // 64
    jj = np.arange(32)[None, None, :]
    future = jj > curb
    forced = ((jj == 0) | (jj == curb) | (jj == curb - 1)) & (~future)
    keep = (~future) & (~forced)
    addc = np.where(forced, 1e4, np.where(future, -1.0, 0.0))
    c["c_keep"] = keep.astype(np.float32).reshape(128, 16 * 32)
    c["c_addc"] = addc.astype(np.float32).reshape(128, 16 * 32)
    ci = (np.arange(128) - 1)[:, None] * 16
    sj = np.arange(32)[None, :] * 64
    ov = ((ci < sj + 64) & (ci + 32 > sj)).astype(np.float32)
    ov[0, :] = 0.0
    c["c_ovl"] = np.concatenate([ov, np.ones((128, 1), np.float32)], axis=1)
    sel = np.zeros((48, 48, 64), np.float32)
    for k in range(48):
        sel[k, k, :] = 1.0
    c["c_sel"] = sel.reshape(48, 48 * 64)
    return c


def _prep_shared(inp):
    f = lambda a: np.ascontiguousarray(np.asarray(a, dtype=np.float32))
    sh = {}
    sh["ada_w"] = f(inp["ada_w"])
    sh["ada_bT"] = f(np.asarray(inp["ada_b"]).reshape(4, 48, 128).transpose(2, 0, 1).reshape(128, 4 * 48))
    sh["norm_gT"] = f(np.asarray(inp["norm_g"]).reshape(16, 8, 128).transpose(2, 0, 1).reshape(128, 16 * 8))
    sh["a_w_in"] = f(inp["a_w_in"])
    sh["a_ln_g"] = f(inp["a_ln_g"])
    sh["a_ln_bT"] = f(np.asarray(inp["a_ln_b"]).reshape(2, 8, 128).transpose(2, 0, 1).reshape(128, 16))
    sh["a_w_s"] = f(inp["a_w_s"])
    sh["a_b_s"] = f(np.asarray(inp["a_b_s"]).reshape(2, 8 * 128))
    sh["a_w_out"] = f(inp["a_w_out"])
    sh["kv_ada_w"] = f(inp["kv_ada_w"])
    sh["kv_ada_bT"] = f(np.asarray(inp["kv_ada_b"]).reshape(16, 128).T)
    sh["kv_norm_gT"] = f(np.asarray(inp["kv_norm_g"]).reshape(8, 128).T)
    sh["kv_w"] = f(inp["kv_w"])
    sh["cmp_posT"] = f(np.asarray(inp["cmp_pos"]).transpose(0, 2, 1))
    sh["cmp_w1"] = f(inp["cmp_w1"])
    sh["cmp_b1T"] = f(np.asarray(inp["cmp_b1"]).reshape(2, 2, 128).transpose(2, 0, 1).reshape(128, 4))
    sh["cmp_w2"] = f(inp["cmp_w2"])
    sh["cmp_b2"] = f(inp["cmp_b2"])
    bw = np.asarray(inp["b_w_in"], dtype=np.float32)
    q = bw[:, :, :1024].reshape(2, 1024, 2, 2, 4, 64)
    q = q.transpose(0, 1, 2, 4, 3, 5).reshape(2, 1024, 1024)
    sh["b_w_q"] = f(q)
    sh["b_w_g"] = f(bw[:, :, 1024:])
    sh["b_w_out"] = f(inp["b_w_out"])
    sh["ff_w_in"] = f(inp["ff_w_in"])
    sh["ff_w_out"] = f(inp["ff_w_out"])
    sh.update(_consts())
    return sh


_CACHE = {}


N_LAYERS = 4


def kernel(**inputs):
    n_layers = N_LAYERS
    if "nc" not in _CACHE:
        _CACHE["nc"] = build_program(n_layers)
    nc = _CACHE["nc"]
    sh = _prep_shared(inputs)
    x = np.asarray(inputs["x"], dtype=np.float32)
    c = np.asarray(inputs["c"], dtype=np.float32)
    in_maps = []
    for core in range(NCORES):
        m = dict(sh)
        m["x"] = np.ascontiguousarray(x[core * BPC:(core + 1) * BPC])
        cc = c[core * BPC:(core + 1) * BPC]
        m["cT"] = np.ascontiguousarray(cc.reshape(BPC, 8, 128).transpose(2, 1, 0))
        in_maps.append(m)
    res = run_bass_kernel_spmd(nc, in_maps, core_ids=list(range(NCORES)))
    out = np.concatenate([r["out"] for r in res.results], axis=0)
    return out.astype(np.float32)
```

```python
import numpy as np
import concourse.bass as bass
import concourse.mybir as mybir
from concourse.bass_utils import run_bass_kernel_spmd

F32 = mybir.dt.float32
BF16 = mybir.dt.bfloat16
AF = mybir.ActivationFunctionType
ALU = mybir.AluOpType

NCORES = 8
D = 1024
T = 2048
TT = 512
NTILE = T // TT
BPC = 2
DFF = 4096
EPS = 1e-6
NEG = -30000.0


class Tl:
    __slots__ = ("w", "r")

    def __init__(self):
        self.w = None
        self.r = {}

    def addr(self, tok):
        k = id(tok[0])
        o = self.r.get(k)
        if o is None or o[1] < tok[1]:
            self.r[k] = tok


class Prog:
    ENG = ("pe", "act", "dve", "pool", "sp")

    def __init__(self, nc):
        self.nc = nc
        self.ops = {e: [] for e in self.ENG}
        self.cnt = {e: 0 for e in self.ENG}
        self.esem = {e: nc.alloc_semaphore("e_" + e) for e in self.ENG}
        self.waited = {e: {} for e in self.ENG}
        self.dcnt = {}
        self.nsem = 0

    def new_sem(self, name):
        self.nsem += 1
        return self.nc.alloc_semaphore("%s_%d" % (name, self.nsem))

    def _deps(self, eng, reads, writes):
        best = {}

        def add(tok):
            s, v = tok
            k = id(s)
            if k not in best or best[k][1] < v:
                best[k] = tok

        for t in reads:
            if t.w is not None:
                add(t.w)
        for t in writes:
            if t.w is not None:
                add(t.w)
            for tok in t.r.values():
                add(tok)
        out = []
        wd = self.waited[eng]
        pes = id(self.esem["pe"])
        for k, (s, v) in best.items():
            if eng == "pe" and k == pes:
                continue
            if wd.get(k, 0) >= v:
                continue
            wd[k] = v
            out.append((s, v))
        return out

    def op(self, eng, fn, reads=(), writes=(), dma_sem=None):
        waits = self._deps(eng, reads, writes)
        if dma_sem is None:
            self.cnt[eng] += 1
            tok = (self.esem[eng], self.cnt[eng])
            inc = (self.esem[eng], 1)
        else:
            k = id(dma_sem)
            self.dcnt[k] = self.dcnt.get(k, 0) + 16
            tok = (dma_sem, self.dcnt[k])
            inc = (dma_sem, 16)
        self.ops[eng].append((fn, waits, inc))
        for t in reads:
            t.addr(tok)
        for t in writes:
            t.w = tok
            t.r = {}
        return tok

    def handover(self, old, new):
        toks = []
        for t in old:
            if t.w is not None:
                toks.append(t.w)
            toks.extend(t.r.values())
        best = {}
        for (s, v) in toks:
            k = id(s)
            if k not in best or best[k][1] < v:
                best[k] = (s, v)
        for t in new:
            for tok in best.values():
                t.addr(tok)

    def emit(self, final_waits):
        nc = self.nc
        engmap = {"pe": "tensor", "act": "scalar", "dve": "vector", "pool": "gpsimd", "sp": "sync"}
        with nc.Block() as block:
            for e in self.ENG:
                ops = self.ops[e]

                def run(engobj, ops=ops, e=e):
                    for (fn, waits, inc) in ops:
                        for (s, v) in waits:
                            engobj.wait_ge(s, v)
                        r = fn(engobj)
                        r.then_inc(inc[0], inc[1])
                    if e == "sp":
                        for (s, v) in final_waits:
                            engobj.wait_ge(s, v)

                getattr(block, engmap[e])(run)


class Buf:
    def __init__(self, t):
        self.t = t
        self.d = Tl()


def build_program(n_layers=4):
    nc = bass.Bass("TRN2", target_bir_lowering=False)
    P = Prog(nc)

    def din(name, shape, dt=F32):
        return nc.dram_tensor(name, list(shape), dt, kind="ExternalInput").ap()

    x_d = din("x", [BPC, T, D])
    out_d = nc.dram_tensor("out", [BPC, T, D], F32, kind="ExternalOutput").ap()
    cT_d = din("cT", [128, 8, BPC])
    ada_w_d = din("ada_w", [4, D, 6 * D])
    ada_b_d = din("ada_bT", [128, 4 * 48])
    norm_g_d = din("norm_gT", [128, 16 * 8])
    a_w_in_d = din("a_w_in", [2, D, 2 * D])
    a_ln_g_d = din("a_ln_g", [2, D])
    a_ln_b_d = din("a_ln_bT", [128, 2 * 8])
    a_w_s_d = din("a_w_s", [2, 8, 128, 128])
    a_b_s_d = din("a_b_s", [2, 8 * 128])
    a_w_out_d = din("a_w_out", [2, D, D])
    kv_ada_w_d = din("kv_ada_w", [D, 2 * D])
    kv_ada_b_d = din("kv_ada_bT", [128, 16])
    kv_norm_g_d = din("kv_norm_gT", [128, 8])
    kv_w_d = din("kv_w", [D, 1536])
    cmp_posT_d = din("cmp_posT", [2, 64, 32])
    cmp_w1_d = din("cmp_w1", [2, 2048, 256])
    cmp_b1_d = din("cmp_b1T", [128, 4])
    cmp_w2_d = din("cmp_w2", [2, 256, 64])
    cmp_b2_d = din("cmp_b2", [2, 64])
    b_w_q_d = din("b_w_q", [2, D, 1024])
    b_w_g_d = din("b_w_g", [2, D, 48])
    b_w_out_d = din("b_w_out", [2, D, D])
    ff_w_in_d = din("ff_w_in", [4, D, DFF])
    ff_w_out_d = din("ff_w_out", [4, DFF, D])
    ident_d = din("c_ident", [128, 128])
    tri_d = din("c_tri", [128, 128])
    causal_d = din("c_causal", [128, 128])
    acausal_d = din("c_acausal", [128, 128])
    cmpb_d = din("c_cmpb", [128, 16 * 128])
    E_d = din("c_E", [128, 16 * 128])
    keep_d = din("c_keep", [128, 16 * 32])
    addc_d = din("c_addc", [128, 16 * 32])
    ovl_d = din("c_ovl", [128, 33])
    sel_d = din("c_sel", [48, 48 * 64])

    def dscr(name, shape):
        return nc.dram_tensor(name, list(shape), BF16).ap()

    s_a_w_in = dscr("s_a_w_in", [2, D, 2 * D])
    s_a_w_out = dscr("s_a_w_out", [2, D, D])
    s_kv_w = dscr("s_kv_w", [D, 1536])
    s_cmp_w1 = dscr("s_cmp_w1", [2, 2048, 256])
    s_b_w_q = dscr("s_b_w_q", [2, D, 1024])
    s_b_w_out = dscr("s_b_w_out", [2, D, D])
    s_ff_w_in = dscr("s_ff_w_in", [4, D, DFF])
    s_ff_w_out = dscr("s_ff_w_out", [4, DFF, D])
    s_wsT = dscr("s_wsT", [2, 128, 1024])
    s_bm = nc.dram_tensor("s_bm", [2, 128, 1024], F32).ap()
    scr_ws_d = [Tl(), Tl()]
    scr_bm_d = [Tl(), Tl()]

    cur = [16512]
    top = nc.sbuf_top

    def sb(name, shape, dt, at=None):
        n = 1
        for s in shape[1:]:
            n *= s
        nbytes = n * (4 if dt == F32 else 2)
        nbytes = (nbytes + 31) // 32 * 32
        if at is None:
            off = cur[0]
            cur[0] += nbytes
            assert cur[0] <= top, ("SBUF overflow", name, cur[0], top)
        else:
            off = at
        return Buf(nc.alloc_sbuf_tensor_at(name, list(shape), dt, offset=off))

    xT = [sb("xT%d" % c, [128, TT], F32) for c in range(8)]
    hT = [sb("hT%d" % c, [128, TT], BF16) for c in range(8)]
    yT = [sb("yT%d" % c, [128, TT], F32) for c in range(8)]
    sq = [sb("sq%d" % i, [128, TT], BF16) for i in range(2)]
    rstd = sb("rstd", [128, TT], F32)
    tmpf = [sb("tmpf%d" % i, [128, TT], F32) for i in range(2)]
    NSLOT = 4
    wslot = [sb("wslot%d" % i, [128, 4096], BF16) for i in range(NSLOT)]
    wsem = [P.new_sem("w") for _ in range(NSLOT)]
    xio_sem = [P.new_sem("xio") for _ in range(4)]
    k_selT = sb("k_selT", [128, 2, T], BF16)
    k_winT = sb("k_winT", [128, 2, T], BF16)
    VW = 16 * 512
    vsel_all = sb("vsel_all", [128, VW], BF16)
    vwin_all = sb("vwin_all", [128, VW], BF16)
    vsel_d = [Tl() for _ in range(16)]
    vwin_d = [Tl() for _ in range(16)]
    vones_d = Tl()
    k_cmpT = sb("k_cmpT", [128, 2, 128], BF16)
    v_cmp = sb("v_cmp", [128, 4 * 128], BF16)
    raw_k = sb("raw_k", [128, 2, 16 + TT], BF16)
    raw_v = sb("raw_v", [128, 2, 16 + TT], BF16)
    ident = sb("ident", [128, 128], F32)
    ident_bf = sb("ident_bf", [128, 128], BF16)
    ones_bf = sb("ones_bf", [128, 128], BF16)
    tri = sb("tri", [128, 128], F32)
    causal = sb("causal", [128, 128], BF16)
    acausal = sb("acausal", [128, 128], BF16)
    cmpb = sb("cmpb", [128, 16, 128], BF16)
    Ec = sb("Ec", [128, 16, 128], BF16)
    selbT = [sb("selbT%d" % g, [128, 128], BF16) for g in range(4)]
    selb_f = [sb("selb_f%d" % g, [128, 32], F32) for g in range(4)]
    keepc = sb("keepc", [128, 16, 32], BF16)
    addcc = sb("addcc", [128, 16, 32], BF16)
    ovl = sb("ovl", [128, 33], BF16)
    selc = sb("selc", [48, 48, 64], BF16)
    cst = sb("cst", [128, BPC, 4, 6, 8], F32)
    cstkv = sb("cstkv", [128, BPC, 2, 8], F32)
    wg = sb("wg", [128, 2, 8, 48], BF16)
    w2k = sb("w2k", [128, 2, 192], BF16)
    w2v = sb("w2v", [128, 2, 64], BF16)
    b2k = sb("b2k", [128, 1], F32)
    b2v = sb("b2v", [1, 64], BF16)
    posb = sb("posb", [128, 2, 2], F32)
    lnb = sb("lnb", [128, 16], F32)
    small = sb("small", [128, 64], F32)
    epsc = sb("epsc", [128, 1], F32)
    hkw = sb("hkw", [128, 2, 2, 4, 32], BF16)
    posT = sb("posT", [64, 2, 32], BF16)
    arena0 = cur[0]

    vtok = [sb("vtok%d" % i, [128, D], F32) for i in range(2)]
    vhat = [sb("vhat%d" % i, [128, D], BF16) for i in range(2)]
    uT = [sb("uT%d" % c, [128, TT], BF16) for c in range(8)]
    oTg = [sb("oTg%d" % c, [128, TT], BF16) for c in range(8)]
    gbc = sb("gbc", [128, D], F32)
    bnst = sb("bnst", [128, 32], F32)
    wsT_l = sb("wsT_l", [128, 8, 128], BF16)
    bm_l = sb("bm_l", [128, 8, 128], F32)
    arena_end = cur[0]
    tilesA = vtok + vhat + uT + oTg + [gbc, bnst, wsT_l, bm_l]
    cur[0] = arena0
    qT_all = sb("qT_all", [128, 8, TT], BF16)
    gsT = sb("gsT", [48, TT], BF16)
    pT = [sb("pT%d" % i, [128, TT], BF16) for i in range(3)]
    o_acc = [sb("o_acc%d" % i, [64, TT], F32) for i in range(4)]
    o_tmp = [sb("o_tmp%d" % i, [64, TT], F32) for i in range(1)]
    rden = [sb("rden%d" % i, [64, TT], F32) for i in range(1)]
    ffac = [sb("ffac%d" % i, [64, TT], F32) for i in range(1)]
    oT_all = sb("oT_all", [128, 8, TT], BF16)
    impb = sb("impb", [128, 4, 64], F32)
    arena_end = max(arena_end, cur[0])
    tilesB = [qT_all] + [gsT] + pT + o_acc + o_tmp + rden + ffac + [oT_all] + [impb]
    cur[0] = arena0
    xio = [sb("xio%d" % s, [128, D], F32) for s in range(4)]
    stage = [sb("stage%d" % i, [128, 2048], F32) for i in range(2)]
    stage_sem = [P.new_sem("stg") for _ in range(2)]
    modT = sb("modT", [128, 5, 48, BPC], F32)
    adab = sb("adab", [128, 5, 48], F32)
    normg = sb("normg", [128, 17, 8], F32)
    wsT_p = sb("wsT_p", [128, 8, 128], BF16)
    bm_p = sb("bm_p", [128, 8, 128], F32)
    arena_end = max(arena_end, cur[0])
    tilesC = xio + stage + [modT, adab, normg, wsT_p, bm_p]
    cur[0] = arena0
    hid = [[sb("hid%d_%d" % (i, c), [128, TT], BF16) for c in range(4)] for i in range(2)]
    relu = [sb("relu%d" % i, [128, TT], F32) for i in range(2)]
    arena_end = max(arena_end, cur[0])
    tilesD = hid[0] + hid[1] + relu
    cur[0] = arena_end
    assert cur[0] <= top, ("SBUF overflow", cur[0], top)
    print("SBUF used", cur[0], "of", top)
    overlays = {"A": tilesA, "B": tilesB, "C": tilesC, "D": tilesD}

    def phase(name):
        old = []
        for k, v in overlays.items():
            if k != name:
                old.extend(t.d for t in v)
        P.handover(old, [t.d for t in overlays[name]])

    psb = [Buf(nc.alloc_psum_tensor("ps%d" % i, [128, 512], F32)) for i in range(8)]
    psi = [0]

    PS_GEN = [0, 1, 2, 3, 7]

    def ps():
        b = psb[PS_GEN[psi[0] % 5]]
        psi[0] += 1
        return b

    acc_i = [0]
    pT_rot = [0]
    pss_i = [0]
    psm_i = [0]

    def ps_s():
        b = psb[pss_i[0] % 3]
        pss_i[0] += 1
        return b

    def ps_m():
        b = psb[(3, 7)[psm_i[0] % 2]]
        psm_i[0] += 1
        return b

    def ps_acc():
        k = acc_i[0] % 3
        acc_i[0] += 1
        return psb[4 + k]


    def vaug(buf, blk, rows=128):
        return buf.t[0:rows, blk * 128:(blk + 1) * 128]

    def mmg(out_ap, pairs, reads, wr, first=True, last=True):
        n = len(pairs)

        def fn(e):
            r = None
            for i, (l, rr) in enumerate(pairs):
                r = e.matmul(out_ap, l, rr, start=(first and i == 0), stop=(last and i == n - 1))
            return r

        P.op("pe", fn, reads=reads, writes=[wr])

    def mm_multi(items, reads, wr):
        def fn(e):
            r = None
            for (o, l, rr) in items:
                r = e.matmul(o, l, rr, start=True, stop=True)
            return r

        P.op("pe", fn, reads=reads, writes=[wr])

    def act(out_ap, in_ap, func, reads, wr, bias=None, scale=None):
        kw = {}
        if bias is not None:
            kw["bias"] = bias
        if scale is not None:
            kw["scale"] = scale
        P.op("act", lambda e: e.activation(out=out_ap, in_=in_ap, func=func, **kw), reads=reads, writes=[wr])

    def tt(eng, out_ap, a, b, op, reads, wr):
        P.op(eng, lambda e: e.tensor_tensor(out=out_ap, in0=a, in1=b, op=op), reads=reads, writes=[wr])

    def stt(eng, out_ap, a, scalar, b, op0, op1, reads, wr):
        P.op(eng, lambda e: e.scalar_tensor_tensor(out=out_ap, in0=a, scalar=scalar, in1=b, op0=op0, op1=op1),
             reads=reads, writes=[wr])

    def ts(eng, out_ap, a, s1, s2, op0, op1, reads, wr):
        if op1 is None:
            P.op(eng, lambda e: e.tensor_scalar(out=out_ap, in0=a, scalar1=s1, scalar2=None, op0=op0),
                 reads=reads, writes=[wr])
        else:
            P.op(eng, lambda e: e.tensor_scalar(out=out_ap, in0=a, scalar1=s1, scalar2=s2, op0=op0, op1=op1),
                 reads=reads, writes=[wr])

    def cp(eng, out_ap, in_ap, reads, wr):
        P.op(eng, lambda e: e.tensor_copy(out=out_ap, in_=in_ap), reads=reads, writes=[wr])

    def dma(eng, out_ap, in_ap, sem, reads, wr):
        P.op(eng, lambda e: e.dma_start(out=out_ap, in_=in_ap), reads=reads, writes=[wr], dma_sem=sem)

    def recip(out_ap, in_ap, reads, wr):
        P.op("dve", lambda e: e.reciprocal(out=out_ap, in_=in_ap), reads=reads, writes=[wr])

    def transp(out_ap, in_ap, idn, reads, wr):
        P.op("pe", lambda e: e.transpose(out_ap, in_ap, idn), reads=reads, writes=[wr])

    def memset(eng, ap, val, wr):
        P.op(eng, lambda e: e.memset(ap, val), writes=[wr])

    _bsem = {}

    def bsem(buf):
        k = id(buf)
        if k not in _bsem:
            _bsem[k] = P.new_sem("b")
        return _bsem[k]

    def ldc(eng, buf, out_ap, src):
        dma(eng, out_ap, src, bsem(buf), [], buf.d)

    def load_x(b, i):
        t0 = i * TT
        for s in range(4):
            dma("pool", xio[s].t[:, :], x_d[b, t0 + s * 128:t0 + (s + 1) * 128, :], xio_sem[s], [], xio[s].d)

    ldc("sp", ident, ident.t[:, :], ident_d)
    ldc("sp", tri, tri.t[:, :], tri_d)
    ldc("pool", keepc, keepc.t[:, :, :], keep_d.rearrange("p (a b) -> p a b", b=32))
    ldc("pool", addcc, addcc.t[:, :, :], addc_d.rearrange("p (a b) -> p a b", b=32))
    ldc("pool", ident_bf, ident_bf.t[:, :], ident_d)
    ldc("pool", causal, causal.t[:, :], causal_d)
    ldc("pool", acausal, acausal.t[:, :], acausal_d)
    ldc("pool", cmpb, cmpb.t[:, :, :], cmpb_d.rearrange("p (a b) -> p a b", b=128))
    ldc("pool", Ec, Ec.t[:, :, :], E_d.rearrange("p (a b) -> p a b", b=128))
    ldc("pool", ovl, ovl.t[:, :], ovl_d)
    ldc("pool", selc, selc.t[:, :, :], sel_d.rearrange("p (a b) -> p a b", b=64))
    memset("dve", ones_bf.t[:, :], 1.0, ones_bf.d)
    for g in range(4):
        memset("dve", selbT[g].t[:, :], 0.0, selbT[g].d)
    memset("dve", epsc.t[:, :], EPS, epsc.d)
    memset("dve", vsel_all.t[:, :], 1.0, vones_d)
    memset("dve", vwin_all.t[:, :], 1.0, vones_d)
    memset("dve", v_cmp.t[:, :], 1.0, vones_d)
    for l in range(2):
        ldc("pool", wg, wg.t[:, l, :, :], b_w_g_d[l].rearrange("(k p) n -> p k n", p=128))
    memset("dve", w2k.t[:, :, :], 0.0, w2k.d)
    ldc("pool", w2k, w2k.t[:, :, 64:128], cmp_w2_d[0].rearrange("(k p) n -> p k n", p=128))
    ldc("pool", w2v, w2v.t[:, :, :], cmp_w2_d[1].rearrange("(k p) n -> p k n", p=128))
    ldc("sp", b2k, b2k.t[0:64, :], cmp_b2_d[0].rearrange("(p o) -> p o", o=1))
    ldc("sp", b2k, b2k.t[64:128, :], cmp_b2_d[0].rearrange("(p o) -> p o", o=1))
    ldc("pool", b2v, b2v.t[:, :], cmp_b2_d[1].rearrange("(o n) -> o n", o=1))
    ldc("sp", lnb, lnb.t[:, :], a_ln_b_d)
    ldc("pool", posT, posT.t[:, :, :], cmp_posT_d.rearrange("k d l -> d k l"))
    ldc("sp", posb, posb.t[:, :, :], cmp_b1_d.rearrange("p (k j) -> p k j", j=2))
    ldc("sp", small, small.t[:, 0:16], cT_d.rearrange("p k b -> p (k b)"))
    ldc("sp", adab, adab.t[:, 0:4, :], ada_b_d.rearrange("p (l c) -> p l c", c=48))
    ldc("sp", adab, adab.t[:, 4, 0:16], kv_ada_b_d)
    ldc("sp", normg, normg.t[:, 0:16, :], norm_g_d.rearrange("p (l c) -> p l c", c=8))
    ldc("sp", normg, normg.t[:, 16, :], kv_norm_g_d)

    load_x(0, 0)
    conv = {}

    def convert(key, src, dst, nelem):
        sem = P.new_sem("cv")
        tl = Tl()
        cols = nelem // 128
        s2 = src.rearrange("(p n) -> p n", p=128) if False else src
        step = 8192
        for c0 in range(0, cols, step):
            c1 = min(cols, c0 + step)
            dma("pool", dst[:, c0:c1], src[:, c0:c1], sem, [], tl)
        conv[key] = tl

    def flat2(ap, n):
        nd = len(ap.shape)
        names = " ".join("d%d" % i for i in range(nd))
        flat = ap.rearrange("%s -> (%s)" % (names, names))
        return flat.rearrange("(p n) -> p n", p=128)

    def conv_w(key, src_ap, dst_ap):
        n = 1
        for s in src_ap.shape:
            n *= s
        convert(key, flat2(src_ap, n), flat2(dst_ap, n), n)

    for l in range(2):
        conv_w(("a_in", l), a_w_in_d[l], s_a_w_in[l])
        conv_w(("a_out", l), a_w_out_d[l], s_a_w_out[l])
        conv_w(("ff_in", l), ff_w_in_d[l], s_ff_w_in[l])
        conv_w(("ff_out", l), ff_w_out_d[l], s_ff_w_out[l])
    conv_w(("kv",), kv_w_d, s_kv_w)
    conv_w(("w1",), cmp_w1_d, s_cmp_w1)
    for l in range(2, 4):
        conv_w(("b_q", l), b_w_q_d[l - 2], s_b_w_q[l - 2])
        conv_w(("b_out", l), b_w_out_d[l - 2], s_b_w_out[l - 2])
        conv_w(("ff_in", l), ff_w_in_d[l], s_ff_w_in[l])
        conv_w(("ff_out", l), ff_w_out_d[l], s_ff_w_out[l])

    act(small.t[:, 16:32], small.t[:, 0:16], AF.Silu, [small.d], small.d)
    cactv = small.t[:, 16:32].rearrange("p (k b) -> p k b", b=BPC)
    bi = 0
    phase("C")
    for l in range(5):
        nblk = 24 if l < 4 else 8
        src = (ada_w_d[l] if l < 4 else kv_ada_w_d)
        for blk in range(nblk):
            st = stage[bi % 2]
            ssem = stage_sem[bi % 2]
            bi += 1
            stv = st.t[:, :].rearrange("p (k n) -> p k n", n=256)
            dma("sp", stv, src[:, blk * 256:(blk + 1) * 256].rearrange("(k p) n -> p k n", p=128), ssem, [], st.d)
            for cc in range(2):
                pb = ps()
                mmg(pb.t[:, 0:BPC], [(stv[:, k, cc * 128:(cc + 1) * 128], cactv[:, k, :]) for k in range(8)],
                    [st.d, small.d], pb.d)
                ch = blk * 2 + cc
                ts("dve", modT.t[:, l, ch, :], pb.t[:, 0:BPC], adab.t[:, l, ch:ch + 1], None, ALU.add, None,
                   [pb.d, adab.d], modT.d)
    for b in range(BPC):
        for l in range(4):
            for (kind, sc_off, gi) in ((0, 8, 0), (3, 32, 2)):
                stt("dve", cst.t[:, b, l, kind, :], modT.t[:, l, sc_off:sc_off + 8, b], 1.0, normg.t[:, l * 4 + gi, :],
                    ALU.add, ALU.mult, [modT.d, normg.d], cst.d)
            for (kind, sh_off) in ((1, 0), (4, 24)):
                cp("dve", cst.t[:, b, l, kind, :], modT.t[:, l, sh_off:sh_off + 8, b], [modT.d], cst.d)
            for (kind, g_off, gi) in ((2, 16, 1), (5, 40, 3)):
                tt("dve", cst.t[:, b, l, kind, :], modT.t[:, l, g_off:g_off + 8, b], normg.t[:, l * 4 + gi, :], ALU.mult,
                   [modT.d, normg.d], cst.d)
        stt("dve", cstkv.t[:, b, 0, :], modT.t[:, 4, 8:16, b], 1.0, normg.t[:, 16, :], ALU.add, ALU.mult,
            [modT.d, normg.d], cstkv.d)
        cp("dve", cstkv.t[:, b, 1, :], modT.t[:, 4, 0:8, b], [modT.d], cstkv.d)

    for l in range(2):
        st = stage[bi % 2]
        ssem = stage_sem[bi % 2]
        bi += 1
        wv = st.t[:, 0:1024].rearrange("p (g s) -> p g s", s=128)
        dma("sp", wv, a_w_s_d[l].rearrange("g t s -> t g s"), ssem, [], st.d)
        tt("dve", wv, wv, tri.t[:, :].unsqueeze(1).to_broadcast([128, 8, 128]), ALU.mult, [st.d, tri.d], st.d)
        for half in range(2):
            pb = ps()
            for gg in range(4):
                g = half * 4 + gg
                transp(pb.t[:, gg * 128:(gg + 1) * 128], wv[:, g, :], ident.t[:, :], [st.d, ident.d], pb.d)
            cp("dve", wsT_p.t[:, half * 4:(half + 1) * 4, :], pb.t[:, :].rearrange("p (g t) -> p g t", t=128), [pb.d], wsT_p.d)
        ldc("sp", bm_p, bm_p.t[:, :, :], a_b_s_d[l].partition_broadcast(128).rearrange("p (g t) -> p g t", t=128))
        for half in range(2):
            pb = ps()
            mmg(pb.t[:, :], [(ones_bf.t[:, :], wsT_p.t[:, half * 4:(half + 1) * 4, :])], [ones_bf.d, wsT_p.d], pb.d)
            for gg in range(4):
                g = half * 4 + gg
                stt("dve", bm_p.t[:, g, :], pb.t[:, gg * 128:(gg + 1) * 128], lnb.t[:, l * 8 + g:l * 8 + g + 1],
                    bm_p.t[:, g, :], ALU.mult, ALU.add, [pb.d, lnb.d, bm_p.d], bm_p.d)
        dma("sp", s_wsT[l], wsT_p.t[:, :, :].rearrange("p g t -> p (g t)"), bsem(wsT_p), [wsT_p.d], scr_ws_d[l])
        dma("sp", s_bm[l], bm_p.t[:, :, :].rearrange("p g t -> p (g t)"), bsem(bm_p), [bm_p.d], scr_bm_d[l])
        wsT_p.d.addr(scr_ws_d[l].w)
        bm_p.d.addr(scr_bm_d[l].w)

    w1tl = conv[("w1",)]
    for kv in range(2):
        for jc in range(2):
            slot = wslot[(kv * 2 + jc) % NSLOT]
            sem = wsem[(kv * 2 + jc) % NSLOT]
            sv = slot.t[0:64, :].rearrange("p (l j) -> p l j", j=128)
            dma("sp", sv, s_cmp_w1[kv].rearrange("(l d) j -> d l j", d=64)[:, :, jc * 128:(jc + 1) * 128], sem, [w1tl], slot.d)
            pb = ps()
            mmg(pb.t[:, 0:1], [(sv[:, l, :], posT.t[:, kv, l:l + 1]) for l in range(32)], [slot.d, posT.d], pb.d)
            tt("dve", posb.t[:, kv, jc:jc + 1], pb.t[:, 0:1], posb.t[:, kv, jc:jc + 1], ALU.add, [pb.d, posb.d], posb.d)

    def wload(slot_i, out_ap, src_ap, dep):
        dma("sp", out_ap, src_ap, wsem[slot_i], [dep], wslot[slot_i].d)

    slot_rr = [0]

    def next_slot():
        i = slot_rr[0] % NSLOT
        slot_rr[0] += 1
        return i

    def rms_stats(src):
        pb = ps()
        for c in range(8):
            s = sq[c % 2]
            act(s.t[:, :], src[c].t[:, :], AF.Square, [src[c].d], s.d)
            mmg(pb.t[:, :], [(ones_bf.t[:, :], s.t[:, :])], [ones_bf.d, s.d], pb.d, first=(c == 0), last=(c == 7))
        act(rstd.t[:, :], pb.t[:, :], AF.Ln, [pb.d, epsc.d], rstd.d, bias=epsc.t[:, 0:1], scale=1.0 / D)
        act(rstd.t[:, :], rstd.t[:, :], AF.Exp, [rstd.d], rstd.d, scale=-0.5)

    def rmsmod(Gap, Sap):
        rms_stats(xT)
        for c in range(8):
            tf = tmpf[c % 2]
            stt("dve", tf.t[:, :], xT[c].t[:, :], Gap[:, c:c + 1], rstd.t[:, :], ALU.mult, ALU.mult,
                [xT[c].d, rstd.d, cst.d, cstkv.d], tf.d)
            act(hT[c].t[:, :], tf.t[:, :], AF.Identity, [tf.d, cst.d, cstkv.d], hT[c].d, bias=Sap[:, c:c + 1], scale=1.0)

    def residual(GGap):
        rms_stats(yT)
        for c in range(8):
            tf = tmpf[c % 2]
            stt("dve", tf.t[:, :], yT[c].t[:, :], GGap[:, c:c + 1], rstd.t[:, :], ALU.mult, ALU.mult,
                [yT[c].d, rstd.d, cst.d], tf.d)
            tt("pool", xT[c].t[:, :], xT[c].t[:, :], tf.t[:, :], ALU.add, [xT[c].d, tf.d], xT[c].d)

    def ffn(b, l):
        phase("D")
        rmsmod(cst.t[:, b, l, 3, :], cst.t[:, b, l, 4, :])
        tin = conv[("ff_in", l)]
        tout = conv[("ff_out", l)]
        hds = [h.d for h in hT]
        slots = {}

        def load(j):
            sa = next_slot()
            sbi = next_slot()
            A = wslot[sa].t[:, :].rearrange("p (k n) -> p k n", n=512)
            B = wslot[sbi].t[:, :].rearrange("p (k n) -> p k n", n=1024)
            wload(sa, A, s_ff_w_in[l][:, j * 512:(j + 1) * 512].rearrange("(k p) n -> p k n", p=128), tin)
            wload(sbi, B, s_ff_w_out[l][j * 512:(j + 1) * 512, :].rearrange("(k p) n -> p k n", p=128), tout)
            slots[j] = (sa, sbi, A, B)

        def hidden(j):
            sa, sbi, A, B = slots[j]
            hb = hid[j % 2]
            for c in range(4):
                pb = ps()
                mmg(pb.t[:, :], [(A[:, k, c * 128:(c + 1) * 128], hT[k].t[:, :]) for k in range(8)],
                    [wslot[sa].d] + hds, pb.d)
                r = relu[c % 2]
                act(r.t[:, :], pb.t[:, :], AF.Relu, [pb.d], r.d)
                tt("pool", hb[c].t[:, :], r.t[:, :], r.t[:, :], ALU.mult, [r.d], hb[c].d)

        def ypart(j):
            sa, sbi, A, B = slots[j]
            hb = hid[j % 2]
            for n in range(8):
                pb = ps()
                mmg(pb.t[:, :], [(B[:, c, n * 128:(n + 1) * 128], hb[c].t[:, :]) for c in range(4)],
                    [wslot[sbi].d] + [h.d for h in hb], pb.d)
                if j == 0:
                    act(yT[n].t[:, :], pb.t[:, :], AF.Copy, [pb.d], yT[n].d)
                else:
                    tt("dve", yT[n].t[:, :], pb.t[:, :], yT[n].t[:, :], ALU.add, [pb.d, yT[n].d], yT[n].d)

        load(0)
        hidden(0)
        for j in range(8):
            if j + 1 < 8:
                load(j + 1)
                hidden(j + 1)
            ypart(j)
        residual(cst.t[:, b, l, 5, :])

    def gmlp(b, l):
        phase("A")
        rmsmod(cst.t[:, b, l, 0, :], cst.t[:, b, l, 1, :])
        tin = conv[("a_in", l)]
        hds = [h.d for h in hT]
        ldc("sp", gbc, gbc.t[:, :], a_ln_g_d[l].partition_broadcast(128))
        dma("sp", wsT_l.t[:, :, :].rearrange("p g t -> p (g t)"), s_wsT[l], bsem(wsT_l), [scr_ws_d[l]], wsT_l.d)
        dma("sp", bm_l.t[:, :, :].rearrange("p g t -> p (g t)"), s_bm[l], bsem(bm_l), [scr_bm_d[l]], bm_l.d)
        for half in range(2):
            si = next_slot()
            W = wslot[si].t[:, :].rearrange("p (k n) -> p k n", n=512)
            wload(si, W, s_a_w_in[l][:, half * 512:(half + 1) * 512].rearrange("(k p) n -> p k n", p=128), tin)
            for cc in range(4):
                n = half * 4 + cc
                pb = ps()
                mmg(pb.t[:, :], [(W[:, k, cc * 128:(cc + 1) * 128], hT[k].t[:, :]) for k in range(8)],
                    [wslot[si].d] + hds, pb.d)
                act(uT[n].t[:, :], pb.t[:, :], AF.Gelu_apprx_tanh, [pb.d], uT[n].d)
        sv = [next_slot(), next_slot()]
        Wv = []
        for half in range(2):
            W = wslot[sv[half]].t[:, :].rearrange("p (k n) -> p k n", n=512)
            wload(sv[half], W, s_a_w_in[l][:, D + half * 512:D + (half + 1) * 512].rearrange("(k p) n -> p k n", p=128), tin)
            Wv.append(W)
        def vmat(s):
            vt = vtok[s % 2]
            for half in range(2):
                pb = ps()
                mmg(pb.t[:, :], [(hT[k].t[:, s * 128:(s + 1) * 128], Wv[half][:, k, :]) for k in range(8)],
                    [wslot[sv[half]].d] + hds, pb.d)
                act(vt.t[:, half * 512:(half + 1) * 512], pb.t[:, :], AF.Gelu_apprx_tanh, [pb.d], vt.d)

        def vrest(s):
            vt = vtok[s % 2]
            vh = vhat[s % 2]
            for half in range(2):
                P.op("dve", lambda e, o=bnst.t[:, half * 6:half * 6 + 6], i=vt.t[:, half * 512:(half + 1) * 512]: e.bn_stats(out=o, in_=i),
                     reads=[vt.d], writes=[bnst.d])
            P.op("dve", lambda e, o=bnst.t[:, 16:18], i=bnst.t[:, 0:12].rearrange("p (a b) -> p a b", b=6): e.bn_aggr(out=o, in_=i),
                 reads=[bnst.d], writes=[bnst.d])
            act(bnst.t[:, 18:19], bnst.t[:, 17:18], AF.Sqrt, [bnst.d, epsc.d], bnst.d, bias=epsc.t[:, 0:1], scale=1.0)
            recip(bnst.t[:, 19:20], bnst.t[:, 18:19], [bnst.d], bnst.d)
            stt("dve", vt.t[:, :], vt.t[:, :], bnst.t[:, 16:17], gbc.t[:, :], ALU.subtract, ALU.mult,
                [vt.d, bnst.d, gbc.d], vt.d)
            act(vh.t[:, :], vt.t[:, :], AF.Identity, [vt.d, bnst.d], vh.d, scale=bnst.t[:, 19:20])
            for half in range(2):
                pb = ps()
                mm_multi([(pb.t[:, gg * 128:(gg + 1) * 128], vh.t[:, (half * 4 + gg) * 128:(half * 4 + gg + 1) * 128],
                           wsT_l.t[:, half * 4 + gg, :]) for gg in range(4)], [vh.d, wsT_l.d], pb.d)
                tf = tmpf[half]
                tt("dve", tf.t[:, :], pb.t[:, :], bm_l.t[:, half * 4:(half + 1) * 4, :].rearrange("p g t -> p (g t)"),
                   ALU.add, [pb.d, bm_l.d], tf.d)
                for gg in range(4):
                    g = half * 4 + gg
                    tt("pool", oTg[g].t[:, s * 128:(s + 1) * 128], tf.t[:, gg * 128:(gg + 1) * 128],
                       uT[g].t[:, s * 128:(s + 1) * 128], ALU.mult, [tf.d, uT[g].d], oTg[g].d)

        vmat(0)
        for s in range(4):
            if s + 1 < 4:
                vmat(s + 1)
            vrest(s)
        tout = conv[("a_out", l)]
        ods = [o.d for o in oTg]
        for half in range(2):
            si = next_slot()
            W = wslot[si].t[:, :].rearrange("p (k n) -> p k n", n=512)
            wload(si, W, s_a_w_out[l][:, half * 512:(half + 1) * 512].rearrange("(k p) n -> p k n", p=128), tout)
            for cc in range(4):
                n = half * 4 + cc
                pb = ps()
                mmg(pb.t[:, :], [(W[:, k, cc * 128:(cc + 1) * 128], oTg[k].t[:, :]) for k in range(8)],
                    [wslot[si].d] + ods, pb.d)
                act(yT[n].t[:, :], pb.t[:, :], AF.Copy, [pb.d], yT[n].d)
        residual(cst.t[:, b, l, 2, :])

    def kvproj(b, i):
        rmsmod(cstkv.t[:, b, 0, :], cstkv.t[:, b, 1, :])
        tkv = conv[("kv",)]
        hds = [h.d for h in hT]
        t0 = i * TT
        for blk in range(3):
            si = next_slot()
            W = wslot[si].t[:, :].rearrange("p (k n) -> p k n", n=512)
            wload(si, W, s_kv_w[:, blk * 512:(blk + 1) * 512].rearrange("(k p) n -> p k n", p=128), tkv)
            for gh in range(2):
                pb = ps()
                mmg(pb.t[:, :], [(W[:, k, gh * 128:(gh + 1) * 128], hT[k].t[:, :]) for k in range(8)], [wslot[si].d] + hds, pb.d)
                if blk == 0:
                    act(raw_k.t[:, gh, 16:16 + TT], pb.t[:, :], AF.Copy, [pb.d], raw_k.d)
                elif blk == 1:
                    act(k_selT.t[:, gh, t0:t0 + TT], pb.t[:, :], AF.Copy, [pb.d], k_selT.d)
                else:
                    act(k_winT.t[:, gh, t0:t0 + TT], pb.t[:, :], AF.Copy, [pb.d], k_winT.d)
            if blk == 0:
                for gh in range(2):
                    pb = ps()
                    mmg(pb.t[:, :], [(W[:, k, 256 + gh * 128:256 + (gh + 1) * 128], hT[k].t[:, :]) for k in range(8)],
                        [wslot[si].d] + hds, pb.d)
                    act(raw_v.t[:, gh, 16:16 + TT], pb.t[:, :], AF.Copy, [pb.d], raw_v.d)
            else:
                vv, vd = (vsel_all, vsel_d) if blk == 1 else (vwin_all, vwin_d)
                for s in range(4):
                    kt = i * 4 + s
                    pb = ps()
                    mmg(pb.t[:, 0:256], [(hT[k].t[:, s * 128:(s + 1) * 128], W[:, k, 256:512]) for k in range(8)],
                        [wslot[si].d] + hds, pb.d)
                    cp("dve", vv.t[:, kt * 512:(kt + 1) * 512].rearrange("p (g c) -> p g c", c=128)[:, :, 0:64],
                       pb.t[:, 0:256].rearrange("p (g d) -> p g d", d=64), [pb.d, vones_d], vd[kt])
        for kv in range(2):
            raw = raw_k if kv == 0 else raw_v
            for jc in range(2):
                si = next_slot()
                sv = wslot[si].t[:, :].rearrange("p (l j) -> p l j", j=128)
                src = s_cmp_w1[kv].rearrange("(l d) j -> d l j", d=64)[:, :, jc * 128:(jc + 1) * 128]
                wload(si, sv[0:64], src, conv[("w1",)])
                wload(si, sv[64:128], src, conv[("w1",)])
                for ph in range(2):
                    pb = ps()
                    mmg(pb.t[:, 0:64].rearrange("p (a m) -> p a m", m=32),
                        [(sv[ph * 64:(ph + 1) * 64, l, :], raw.t[ph * 64:(ph + 1) * 64, :, l:l + 16 * 31 + 1:16]) for l in range(32)],
                        [wslot[si].d, raw.d], pb.d)
                    for gh in range(2):
                        act(hkw.t[:, jc, kv, gh * 2 + ph, :], pb.t[:, gh * 32:(gh + 1) * 32], AF.Gelu_apprx_tanh,
                            [pb.d, posb.d], hkw.d, bias=posb.t[:, kv, jc:jc + 1], scale=1.0)
        for gh in range(2):
            pb = ps()
            pairs = []
            for ph in range(2):
                for jc in range(2):
                    lw = w2k.t[:, jc, 64:192] if ph == 0 else w2k.t[:, jc, 0:128]
                    pairs.append((lw, hkw.t[:, jc, 0, gh * 2 + ph, :]))
            mmg(pb.t[:, 0:32], pairs, [w2k.d, hkw.d], pb.d)
            act(k_cmpT.t[:, gh, i * 32:(i + 1) * 32], pb.t[:, 0:32], AF.Identity, [pb.d, b2k.d], k_cmpT.d,
                bias=b2k.t[:, 0:1], scale=1.0)
        pb = ps()
        for g in range(4):
            pairs = [(hkw.t[:, jc, 1, g, :], w2v.t[:, jc, :]) for jc in range(2)]
            pairs.append((ones_bf.t[0:1, 0:32], b2v.t[0:1, :]))
            mmg(pb.t[0:32, g * 64:(g + 1) * 64], pairs, [hkw.d, w2v.d, b2v.d, ones_bf.d], pb.d)
        cp("dve", v_cmp.t[i * 32:(i + 1) * 32, :].rearrange("p (g c) -> p g c", c=128)[:, :, 0:64],
           pb.t[0:32, 0:256].rearrange("p (g d) -> p g d", d=64), [pb.d, vones_d], v_cmp.d)
        cp("pool", raw_k.t[:, :, 0:16], raw_k.t[:, :, TT:TT + 16], [raw_k.d], raw_k.d)
        cp("pool", raw_v.t[:, :, 0:16], raw_v.t[:, :, TT:TT + 16], [raw_v.d], raw_v.d)

    def branch_finish(psa, g, br, s, oa, first, guard, final=False):
        rd = rden[0]
        ff = ffac[0]
        ot = o_tmp[0]
        if guard:
            ts("dve", rd.t[:, :], psa.t[64:128, :], 1e-30, None, ALU.max, None, [psa.d], rd.d)
            act(rd.t[:, :], rd.t[:, :], AF.Ln, [rd.d], rd.d)
        else:
            act(rd.t[:, :], psa.t[64:128, :], AF.Ln, [psa.d], rd.d)
        act(rd.t[:, :], rd.t[:, :], AF.Exp, [rd.d], rd.d, scale=-1.0)
        pg = ps_m()
        mm_multi([(pg.t[0:64, hh * 128:(hh + 1) * 128], selc.t[:, (g * 4 + hh) * 3 + br, :], gsT.t[:, s * 128:(s + 1) * 128])
                  for hh in range(4)], [selc.d, gsT.d], pg.d)
        tt("dve", ff.t[:, :], pg.t[0:64, :], rd.t[:, :], ALU.mult, [pg.d, rd.d], ff.d)
        if first:
            tt("dve", oa.t[:, :], psa.t[0:64, :], ff.t[:, :], ALU.mult, [psa.d, ff.d], oa.d)
        else:
            tt("dve", ot.t[:, :], psa.t[0:64, :], ff.t[:, :], ALU.mult, [psa.d, ff.d], ot.d)
            if final:
                oav = oa.t[:, :].rearrange("p (j two t) -> p j two t", two=2, t=128)
                otv = ot.t[:, :].rearrange("p (j two t) -> p j two t", two=2, t=128)
                for half in range(2):
                    tt("pool" if half == 0 else "dve",
                       oT_all.t[half * 64:(half + 1) * 64, g * 2:g * 2 + 2, s * 128:(s + 1) * 128],
                       oav[:, :, half, :], otv[:, :, half, :], ALU.add, [oa.d, ot.d], oT_all.d)
            else:
                tt("pool", oa.t[:, :], oa.t[:, :], ot.t[:, :], ALU.add, [oa.d, ot.d], oa.d)

    def nsa(b, l, i):
        phase("B")
        j = l - 2
        rmsmod(cst.t[:, b, l, 0, :], cst.t[:, b, l, 1, :])
        hds = [h.d for h in hT]
        tq = conv[("b_q", l)]
        for half in range(2):
            si = next_slot()
            W = wslot[si].t[:, :].rearrange("p (k n) -> p k n", n=512)
            wload(si, W, s_b_w_q[j][:, half * 512:(half + 1) * 512].rearrange("(k p) n -> p k n", p=128), tq)
            for cc in range(4):
                n = half * 4 + cc
                pb = ps()
                mmg(pb.t[:, :], [(W[:, k, cc * 128:(cc + 1) * 128], hT[k].t[:, :]) for k in range(8)], [wslot[si].d] + hds, pb.d)
                act(qT_all.t[:, n, :], pb.t[:, :], AF.Identity, [pb.d], qT_all.d, scale=0.125)
        pb = ps()
        mmg(pb.t[0:48, :], [(wg.t[:, j, k, :], hT[k].t[:, :]) for k in range(8)], [wg.d] + hds, pb.d)
        act(gsT.t[:, :], pb.t[0:48, :], AF.Sigmoid, [pb.d], gsT.d)
        ncc = 32 * (i + 1)
        b4 = lambda ap: ap.unsqueeze(1).to_broadcast([ap.shape[0], 4, 128])

        def make_scores(pb, rows, lhsT, rhs4, extra):
            n = len(extra)
            full = pb.t[0:rows, :].rearrange("p (a t) -> p a t", t=128)
            ex = list(extra)

            def fn(e):
                r = e.matmul(full, lhsT, rhs4, start=True, stop=(n == 0), skip_group_check=True)
                for ei, (el, er) in enumerate(ex):
                    r = e.matmul(full, el, er, start=False, stop=(ei == n - 1), skip_group_check=True)
                return r
            return fn

        units = []
        for s in range(4):
            qt = i * 4 + s
            use_sel = qt >= 8
            kt0 = max(0, qt - 4)
            for g in range(4):
                units.append(("cmp", s, g, 0, True, True))
                for kt in range(kt0, qt + 1):
                    units.append(("win", s, g, kt, kt == kt0, kt == qt))
            for g in range(4):
                for kt in range(qt + 1):
                    units.append(("sel", s, g, kt, kt == 0, kt == qt))
        state = {}
        accs = {}
        deferred = []

        def emit_scores(u):
            kind, s, g, kt, ufirst, ulast = u
            qt = i * 4 + s
            gh, ph = g // 2, g % 2
            pr = slice(ph * 64, (ph + 1) * 64)
            qds = [qT_all.d]
            rhss = qT_all.t[pr, gh * 4:(gh + 1) * 4, s * 128:(s + 1) * 128]
            pb = ps_s()
            if kind == "cmp":
                P.op("pe", make_scores(pb, ncc, k_cmpT.t[pr, gh, 0:ncc], rhss, [(ident_bf.t[:, 0:ncc], b4(cmpb.t[:, qt, :]))]),
                     reads=qds + [k_cmpT.d, ident_bf.d, cmpb.d], writes=[pb.d])
            elif kind == "sel":
                extra = []
                rd = list(qds) + [k_selT.d]
                if qt >= 8:
                    extra.append((Ec.t[:, kt, :], b4(selbT[g].t[:, :])))
                    rd += [Ec.d, selbT[g].d]
                if kt == qt:
                    extra.append((ident_bf.t[:, :], b4(causal.t[:, :])))
                    rd += [ident_bf.d, causal.d]
                P.op("pe", make_scores(pb, 128, k_selT.t[pr, gh, kt * 128:(kt + 1) * 128], rhss, extra), reads=rd, writes=[pb.d])
            else:
                extra = []
                rd = list(qds) + [k_winT.d]
                if kt == qt:
                    extra.append((ident_bf.t[:, :], b4(causal.t[:, :])))
                    rd += [ident_bf.d, causal.d]
                if kt == qt - 4:
                    extra.append((ident_bf.t[:, :], b4(acausal.t[:, :])))
                    rd += [ident_bf.d, acausal.d]
                P.op("pe", make_scores(pb, 128, k_winT.t[pr, gh, kt * 128:(kt + 1) * 128], rhss, extra), reads=rd, writes=[pb.d])
            state[u] = pb

        def emit_post(u):
            kind, s, g, kt, ufirst, ulast = u
            qt = i * 4 + s
            pb = state.pop(u)
            oa = o_acc[g]
            rows = ncc if kind == "cmp" else 128
            p = pT[psi[0] % 3]
            psi[0] += 0
            pT_rot[0] += 1
            p = pT[pT_rot[0] % 3]
            act(p.t[0:rows, :], pb.t[0:rows, :], AF.Exp, [pb.d], p.d)
            key = (kind, s, g)
            if ufirst:
                accs[key] = ps_acc()
            psa = accs[key]
            if kind == "cmp":
                lw, lwd = vaug(v_cmp, g, rows), [v_cmp.d, vones_d]
            elif kind == "sel":
                lw, lwd = vaug(vsel_all, kt * 4 + g), [vsel_d[kt], vones_d]
            else:
                lw, lwd = vaug(vwin_all, kt * 4 + g), [vwin_d[kt], vones_d]
            mmg(psa.t[:, :], [(lw, p.t[0:rows, :])], lwd + [p.d], psa.d, first=ufirst, last=ulast)
            if not ulast:
                return
            del accs[key]
            if kind == "cmp":
                use_sel = qt >= 8
                if use_sel:
                    pim = ps_m()
                    mm_multi([(pim.t[:, hh * 64:hh * 64 + 33], p.t[0:ncc, hh * 128:(hh + 1) * 128], ovl.t[0:ncc, :]) for hh in range(4)],
                             [p.d, ovl.d], pim.d)
                branch_finish(psa, g, 0, s, oa, True, qt == 0)
                if use_sel:
                    pv = pim.t[:, 0:256].rearrange("p (h c) -> p h c", c=64)
                    recip(impb.t[:, 0, 32:36], pv[:, :, 32], [pim.d], impb.d)
                    ts("dve", impb.t[:, 1, 0:32], pv[:, 0, 0:32], impb.t[:, 0, 32:33], None, ALU.mult, None, [pim.d, impb.d], impb.d)
                    for hh in range(1, 4):
                        stt("dve", impb.t[:, 1, 0:32], pv[:, hh, 0:32], impb.t[:, 0, 32 + hh:33 + hh], impb.t[:, 1, 0:32],
                            ALU.mult, ALU.add, [pim.d, impb.d], impb.d)
                    tt("dve", impb.t[:, 1, 0:32], impb.t[:, 1, 0:32], keepc.t[:, qt, :], ALU.mult, [impb.d, keepc.d], impb.d)
                    tt("dve", impb.t[:, 1, 0:32], impb.t[:, 1, 0:32], addcc.t[:, qt, :], ALU.add, [impb.d, addcc.d], impb.d)
                    P.op("dve", lambda e: e.max(out=impb.t[:, 2, 0:8], in_=impb.t[:, 1, 0:32]), reads=[impb.d], writes=[impb.d])
                    P.op("dve", lambda e: e.match_replace(out=impb.t[:, 3, 0:32], in_to_replace=impb.t[:, 2, 0:8],
                                                          in_values=impb.t[:, 1, 0:32], imm_value=-2.0), reads=[impb.d], writes=[impb.d])
                    P.op("dve", lambda e: e.max(out=impb.t[:, 2, 8:16], in_=impb.t[:, 3, 0:32]), reads=[impb.d], writes=[impb.d])
                    sb_src = selb_f[g]
                    ts("dve", sb_src.t[:, :], impb.t[:, 1, 0:32], impb.t[:, 2, 15:16], NEG, ALU.is_lt, ALU.mult, [impb.d], sb_src.d)

                    def fin(g=g, sb_src=sb_src):
                        ptr = ps_m()
                        transp(ptr.t[0:32, 0:128], sb_src.t[:, :], ident.t[:, :], [sb_src.d, ident.d], ptr.d)
                        cp("dve", selbT[g].t[0:32, :], ptr.t[0:32, 0:128], [ptr.d], selbT[g].d)
                    deferred.append([4, (s, g), fin])
            elif kind == "win":
                branch_finish(psa, g, 2, s, oa, False, False)
            else:
                branch_finish(psa, g, 1, s, oa, False, False, final=True)

        def run_deferred(force_key=None):
            keep = []
            for item in deferred:
                if item[0] <= 0 or (force_key is not None and item[1] == force_key):
                    item[2]()
                else:
                    keep.append(item)
            deferred[:] = keep

        def scores_checked(u):
            if u[0] == "sel" and u[3] == 0:
                run_deferred(force_key=(u[1], u[2]))
            emit_scores(u)

        LOOK = 2
        for idx in range(min(LOOK, len(units))):
            scores_checked(units[idx])
        for idx in range(len(units)):
            if idx + LOOK < len(units):
                scores_checked(units[idx + LOOK])
            for item in deferred:
                item[0] -= 1
            run_deferred()
            emit_post(units[idx])
        run_deferred()
        for item in list(deferred):
            item[2]()
        deferred[:] = []
        tout = conv[("b_out", l)]
        ods = [oT_all.d]
        for half in range(2):
            si = next_slot()
            W = wslot[si].t[:, :].rearrange("p (k n) -> p k n", n=512)
            wload(si, W, s_b_w_out[j][:, half * 512:(half + 1) * 512].rearrange("(k p) n -> p k n", p=128), tout)
            for cc in range(4):
                n = half * 4 + cc
                pb = ps()
                mmg(pb.t[:, :], [(W[:, k, cc * 128:(cc + 1) * 128], oT_all.t[:, k, :]) for k in range(8)], [wslot[si].d] + ods, pb.d)
                act(yT[n].t[:, :], pb.t[:, :], AF.Copy, [pb.d], yT[n].d)
        residual(cst.t[:, b, l, 2, :])

    nst = [0]
    for b in range(BPC):
        memset("pool", raw_k.t[:, :, 0:16], 0.0, raw_k.d)
        memset("pool", raw_v.t[:, :, 0:16], 0.0, raw_v.d)
        for i in range(NTILE):
            t0 = i * TT
            phase("C")
            if not (b == 0 and i == 0):
                load_x(b, i)
            for c in range(8):
                pb = ps()
                for s in range(4):
                    transp(pb.t[:, s * 128:(s + 1) * 128], xio[s].t[:, c * 128:(c + 1) * 128], ident.t[:, :], [xio[s].d, ident.d], pb.d)
                act(xT[c].t[:, :], pb.t[:, :], AF.Copy, [pb.d], xT[c].d)
            for l in range(min(2, n_layers)):
                gmlp(b, l)
                ffn(b, l)
            if n_layers > 2:
                kvproj(b, i)
                for l in range(2, n_layers):
                    nsa(b, l, i)
                    ffn(b, l)
            phase("C")
            for s in range(4):
                for half in range(2):
                    pb = ps()
                    for cc in range(4):
                        c = half * 4 + cc
                        transp(pb.t[:, cc * 128:(cc + 1) * 128], xT[c].t[:, s * 128:(s + 1) * 128], ident.t[:, :], [xT[c].d, ident.d], pb.d)
                    act(xio[s].t[:, half * 512:(half + 1) * 512], pb.t[:, :], AF.Copy, [pb.d], xio[s].d)
                dma("pool", out_d[b, t0 + s * 128:t0 + (s + 1) * 128, :], xio[s].t[:, :], xio_sem[s], [xio[s].d], xio[s].d)
                nst[0] += 1
    finals = []
    for s in range(4):
        finals.append((xio_sem[s], P.dcnt[id(xio_sem[s])]))
    print("ops:", {e: len(v) for e, v in P.ops.items()})
    P.emit(finals)
    return nc


def _consts():
    c = {}
    c["c_ident"] = np.eye(128, dtype=np.float32)
    tl = np.arange(128)
    c["c_tri"] = (tl[None, :] <= tl[:, None]).astype(np.float32)
    c["c_causal"] = np.where(tl[:, None] <= tl[None, :], 0.0, NEG).astype(np.float32)
    c["c_acausal"] = np.where(tl[:, None] > tl[None, :], 0.0, NEG).astype(np.float32)
    cc = np.arange(128)[:, None, None]
    qt = np.arange(16)[None, :, None]
    t = qt * 128 + tl[None, None, :]
    valid = (cc >= 1) & (16 * (cc - 1) + 31 <= t)
    c["c_cmpb"] = np.where(valid, 0.0, NEG).astype(np.float32).reshape(128, 16 * 128)
    j = np.arange(32)[:, None, None]
    key = np.arange(16)[None, :, None] * 128 + tl[None, None, :]
    Ef = np.zeros((128, 16 * 128), np.float32)
    Ef[0:32] = ((key // 64) == j).astype(np.float32).reshape(32, 16 * 128)
    c["c_E"] = Ef
    tq = np.arange(16)[None, :, None] * 128 + tl[:, None, None]
    curb = tq // 64
    jj = np.arange(32)[None, None, :]
    future = jj > curb
    forced = ((jj == 0) | (jj == curb) | (jj == curb - 1)) & (~future)
    keep = (~future) & (~forced)
    addc = np.where(forced, 1e4, np.where(future, -1.0, 0.0))
    c["c_keep"] = keep.astype(np.float32).reshape(128, 16 * 32)
    c["c_addc"] = addc.astype(np.float32).reshape(128, 16 * 32)
    ci = (np.arange(128) - 1)[:, None] * 16
    sj = np.arange(32)[None, :] * 64
    ov = ((ci < sj + 64) & (ci + 32 > sj)).astype(np.float32)
    ov[0, :] = 0.0
    c["c_ovl"] = np.concatenate([ov, np.ones((128, 1), np.float32)], axis=1)
    sel = np.zeros((48, 48, 64), np.float32)
    for k in range(48):
        sel[k, k, :] = 1.0
    c["c_sel"] = sel.reshape(48, 48 * 64)
    return c


def _prep_shared(inp):
    f = lambda a: np.ascontiguousarray(np.asarray(a, dtype=np.float32))
    sh = {}
    sh["ada_w"] = f(inp["ada_w"])
    sh["ada_bT"] = f(np.asarray(inp["ada_b"]).reshape(4, 48, 128).transpose(2, 0, 1).reshape(128, 4 * 48))
    sh["norm_gT"] = f(np.asarray(inp["norm_g"]).reshape(16, 8, 128).transpose(2, 0, 1).reshape(128, 16 * 8))
    sh["a_w_in"] = f(inp["a_w_in"])
    sh["a_ln_g"] = f(inp["a_ln_g"])
    sh["a_ln_bT"] = f(np.asarray(inp["a_ln_b"]).reshape(2, 8, 128).transpose(2, 0, 1).reshape(128, 16))
    sh["a_w_s"] = f(inp["a_w_s"])
    sh["a_b_s"] = f(np.asarray(inp["a_b_s"]).reshape(2, 8 * 128))
    sh["a_w_out"] = f(inp["a_w_out"])
    sh["kv_ada_w"] = f(inp["kv_ada_w"])
    sh["kv_ada_bT"] = f(np.asarray(inp["kv_ada_b"]).reshape(16, 128).T)
    sh["kv_norm_gT"] = f(np.asarray(inp["kv_norm_g"]).reshape(8, 128).T)
    sh["kv_w"] = f(inp["kv_w"])
    sh["cmp_posT"] = f(np.asarray(inp["cmp_pos"]).transpose(0, 2, 1))
    sh["cmp_w1"] = f(inp["cmp_w1"])
    sh["cmp_b1T"] = f(np.asarray(inp["cmp_b1"]).reshape(2, 2, 128).transpose(2, 0, 1).reshape(128, 4))
    sh["cmp_w2"] = f(inp["cmp_w2"])
    sh["cmp_b2"] = f(inp["cmp_b2"])
    bw = np.asarray(inp["b_w_in"], dtype=np.float32)
    q = bw[:, :, :1024].reshape(2, 1024, 2, 2, 4, 64)
    q = q.transpose(0, 1, 2, 4, 3, 5).reshape(2, 1024, 1024)
    sh["b_w_q"] = f(q)
    sh["b_w_g"] = f(bw[:, :, 1024:])
    sh["b_w_out"] = f(inp["b_w_out"])
    sh["ff_w_in"] = f(inp["ff_w_in"])
    sh["ff_w_out"] = f(inp["ff_w_out"])
    sh.update(_consts())
    return sh


_CACHE = {}


N_LAYERS = 4


def kernel(**inputs):
    n_layers = N_LAYERS
    if "nc" not in _CACHE:
        _CACHE["nc"] = build_program(n_layers)
    nc = _CACHE["nc"]
    sh = _prep_shared(inputs)
    x = np.asarray(inputs["x"], dtype=np.float32)
    c = np.asarray(inputs["c"], dtype=np.float32)
    in_maps = []
    for core in range(NCORES):
        m = dict(sh)
        m["x"] = np.ascontiguousarray(x[core * BPC:(core + 1) * BPC])
        cc = c[core * BPC:(core + 1) * BPC]
        m["cT"] = np.ascontiguousarray(cc.reshape(BPC, 8, 128).transpose(2, 1, 0))
        in_maps.append(m)
    res = run_bass_kernel_spmd(nc, in_maps, core_ids=list(range(NCORES)))
    out = np.concatenate([r["out"] for r in res.results], axis=0)
    return out.astype(np.float32)
```

```python
import numpy as np
import concourse.bass as bass
import concourse.mybir as mybir
from concourse.bass_utils import run_bass_kernel_spmd

F32 = mybir.dt.float32
BF16 = mybir.dt.bfloat16
AF = mybir.ActivationFunctionType
ALU = mybir.AluOpType

NCORES = 8
D = 1024
T = 2048
TT = 512
NTILE = T // TT
BPC = 2
DFF = 4096
EPS = 1e-6
NEG = -30000.0


class Tl:
    __slots__ = ("w", "r")

    def __init__(self):
        self.w = None
        self.r = {}

    def addr(self, tok):
        k = id(tok[0])
        o = self.r.get(k)
        if o is None or o[1] < tok[1]:
            self.r[k] = tok


class Prog:
    ENG = ("pe", "act", "dve", "pool", "sp")

    def __init__(self, nc):
        self.nc = nc
        self.ops = {e: [] for e in self.ENG}
        self.cnt = {e: 0 for e in self.ENG}
        self.esem = {e: nc.alloc_semaphore("e_" + e) for e in self.ENG}
        self.waited = {e: {} for e in self.ENG}
        self.dcnt = {}
        self.nsem = 0

    def new_sem(self, name):
        self.nsem += 1
        return self.nc.alloc_semaphore("%s_%d" % (name, self.nsem))

    def _deps(self, eng, reads, writes):
        best = {}

        def add(tok):
            s, v = tok
            k = id(s)
            if k not in best or best[k][1] < v:
                best[k] = tok

        for t in reads:
            if t.w is not None:
                add(t.w)
        for t in writes:
            if t.w is not None:
                add(t.w)
            for tok in t.r.values():
                add(tok)
        out = []
        wd = self.waited[eng]
        pes = id(self.esem["pe"])
        for k, (s, v) in best.items():
            if eng == "pe" and k == pes:
                continue
            if wd.get(k, 0) >= v:
                continue
            wd[k] = v
            out.append((s, v))
        return out

    def op(self, eng, fn, reads=(), writes=(), dma_sem=None):
        waits = self._deps(eng, reads, writes)
        if dma_sem is None:
            self.cnt[eng] += 1
            tok = (self.esem[eng], self.cnt[eng])
            inc = (self.esem[eng], 1)
        else:
            k = id(dma_sem)
            self.dcnt[k] = self.dcnt.get(k, 0) + 16
            tok = (dma_sem, self.dcnt[k])
            inc = (dma_sem, 16)
        self.ops[eng].append((fn, waits, inc))
        for t in reads:
            t.addr(tok)
        for t in writes:
            t.w = tok
            t.r = {}
        return tok

    def handover(self, old, new):
        toks = []
        for t in old:
            if t.w is not None:
                toks.append(t.w)
            toks.extend(t.r.values())
        best = {}
        for (s, v) in toks:
            k = id(s)
            if k not in best or best[k][1] < v:
                best[k] = (s, v)
        for t in new:
            for tok in best.values():
                t.addr(tok)

    def emit(self, final_waits):
        nc = self.nc
        engmap = {"pe": "tensor", "act": "scalar", "dve": "vector", "pool": "gpsimd", "sp": "sync"}
        with nc.Block() as block:
            for e in self.ENG:
                ops = self.ops[e]

                def run(engobj, ops=ops, e=e):
                    for (fn, waits, inc) in ops:
                        for (s, v) in waits:
                            engobj.wait_ge(s, v)
                        r = fn(engobj)
                        r.then_inc(inc[0], inc[1])
                    if e == "sp":
                        for (s, v) in final_waits:
                            engobj.wait_ge(s, v)

                getattr(block, engmap[e])(run)


class Buf:
    def __init__(self, t):
        self.t = t
        self.d = Tl()


def build_program(n_layers=4):
    nc = bass.Bass("TRN2", target_bir_lowering=False)
    P = Prog(nc)

    def din(name, shape, dt=F32):
        return nc.dram_tensor(name, list(shape), dt, kind="ExternalInput").ap()

    x_d = din("x", [BPC, T, D])
    out_d = nc.dram_tensor("out", [BPC, T, D], F32, kind="ExternalOutput").ap()
    cT_d = din("cT", [128, 8, BPC])
    ada_w_d = din("ada_w", [4, D, 6 * D])
    ada_b_d = din("ada_bT", [128, 4 * 48])
    norm_g_d = din("norm_gT", [128, 16 * 8])
    a_w_in_d = din("a_w_in", [2, D, 2 * D])
    a_ln_g_d = din("a_ln_g", [2, D])
    a_ln_b_d = din("a_ln_bT", [128, 2 * 8])
    a_w_s_d = din("a_w_s", [2, 8, 128, 128])
    a_b_s_d = din("a_b_s", [2, 8 * 128])
    a_w_out_d = din("a_w_out", [2, D, D])
    kv_ada_w_d = din("kv_ada_w", [D, 2 * D])
    kv_ada_b_d = din("kv_ada_bT", [128, 16])
    kv_norm_g_d = din("kv_norm_gT", [128, 8])
    kv_w_d = din("kv_w", [D, 1536])
    cmp_posT_d = din("cmp_posT", [2, 64, 32])
    cmp_w1_d = din("cmp_w1", [2, 2048, 256])
    cmp_b1_d = din("cmp_b1T", [128, 4])
    cmp_w2_d = din("cmp_w2", [2, 256, 64])
    cmp_b2_d = din("cmp_b2", [2, 64])
    b_w_q_d = din("b_w_q", [2, D, 1024])
    b_w_g_d = din("b_w_g", [2, D, 48])
    b_w_out_d = din("b_w_out", [2, D, D])
    ff_w_in_d = din("ff_w_in", [4, D, DFF])
    ff_w_out_d = din("ff_w_out", [4, DFF, D])
    ident_d = din("c_ident", [128, 128])
    tri_d = din("c_tri", [128, 128])
    causal_d = din("c_causal", [128, 128])
    acausal_d = din("c_acausal", [128, 128])
    cmpb_d = din("c_cmpb", [128, 16 * 128])
    E_d = din("c_E", [128, 16 * 128])
    keep_d = din("c_keep", [128, 16 * 32])
    addc_d = din("c_addc", [128, 16 * 32])
    ovl_d = din("c_ovl", [128, 33])
    sel_d = din("c_sel", [48, 48 * 64])

    def dscr(name, shape):
        return nc.dram_tensor(name, list(shape), BF16).ap()

    s_a_w_in = dscr("s_a_w_in", [2, D, 2 * D])
    s_a_w_out = dscr("s_a_w_out", [2, D, D])
    s_kv_w = dscr("s_kv_w", [D, 1536])
    s_cmp_w1 = dscr("s_cmp_w1", [2, 2048, 256])
    s_b_w_q = dscr("s_b_w_q", [2, D, 1024])
    s_b_w_out = dscr("s_b_w_out", [2, D, D])
    s_ff_w_in = dscr("s_ff_w_in", [4, D, DFF])
    s_ff_w_out = dscr("s_ff_w_out", [4, DFF, D])
    s_wsT = dscr("s_wsT", [2, 128, 1024])
    s_bm = nc.dram_tensor("s_bm", [2, 128, 1024], F32).ap()
    scr_ws_d = [Tl(), Tl()]
    scr_bm_d = [Tl(), Tl()]

    cur = [16512]
    top = nc.sbuf_top

    def sb(name, shape, dt, at=None):
        n = 1
        for s in shape[1:]:
            n *= s
        nbytes = n * (4 if dt == F32 else 2)
        nbytes = (nbytes + 31) // 32 * 32
        if at is None:
            off = cur[0]
            cur[0] += nbytes
            assert cur[0] <= top, ("SBUF overflow", name, cur[0], top)
        else:
            off = at
        return Buf(nc.alloc_sbuf_tensor_at(name, list(shape), dt, offset=off))

    xT = [sb("xT%d" % c, [128, TT], F32) for c in range(8)]
    hT = [sb("hT%d" % c, [128, TT], BF16) for c in range(8)]
    yT = [sb("yT%d" % c, [128, TT], F32) for c in range(8)]
    sq = [sb("sq%d" % i, [128, TT], BF16) for i in range(2)]
    rstd = sb("rstd", [128, TT], F32)
    tmpf = [sb("tmpf%d" % i, [128, TT], F32) for i in range(2)]
    NSLOT = 4
    wslot = [sb("wslot%d" % i, [128, 4096], BF16) for i in range(NSLOT)]
    wsem = [P.new_sem("w") for _ in range(NSLOT)]
    xio_sem = [P.new_sem("xio") for _ in range(4)]
    k_selT = sb("k_selT", [128, 2, T], BF16)
    k_winT = sb("k_winT", [128, 2, T], BF16)
    VW = 16 * 512
    vsel_all = sb("vsel_all", [128, VW], BF16)
    vwin_all = sb("vwin_all", [128, VW], BF16)
    vsel_d = [Tl() for _ in range(16)]
    vwin_d = [Tl() for _ in range(16)]
    vones_d = Tl()
    k_cmpT = sb("k_cmpT", [128, 2, 128], BF16)
    v_cmp = sb("v_cmp", [128, 4 * 128], BF16)
    raw_k = sb("raw_k", [128, 2, 16 + TT], BF16)
    raw_v = sb("raw_v", [128, 2, 16 + TT], BF16)
    ident = sb("ident", [128, 128], F32)
    ident_bf = sb("ident_bf", [128, 128], BF16)
    ones_bf = sb("ones_bf", [128, 128], BF16)
    tri = sb("tri", [128, 128], F32)
    causal = sb("causal", [128, 128], BF16)
    acausal = sb("acausal", [128, 128], BF16)
    cmpb = sb("cmpb", [128, 16, 128], BF16)
    Ec = sb("Ec", [128, 16, 128], BF16)
    selbT = [sb("selbT%d" % g, [128, 128], BF16) for g in range(4)]
    selb_f = [sb("selb_f%d" % g, [128, 32], F32) for g in range(4)]
    keepc = sb("keepc", [128, 16, 32], BF16)
    addcc = sb("addcc", [128, 16, 32], BF16)
    ovl = sb("ovl", [128, 33], BF16)
    selc = sb("selc", [48, 48, 64], BF16)
    cst = sb("cst", [128, BPC, 4, 6, 8], F32)
    cstkv = sb("cstkv", [128, BPC, 2, 8], F32)
    wg = sb("wg", [128, 2, 8, 48], BF16)
    w2k = sb("w2k", [128, 2, 192], BF16)
    w2v = sb("w2v", [128, 2, 64], BF16)
    b2k = sb("b2k", [128, 1], F32)
    b2v = sb("b2v", [1, 64], BF16)
    posb = sb("posb", [128, 2, 2], F32)
    lnb = sb("lnb", [128, 16], F32)
    small = sb("small", [128, 64], F32)
    epsc = sb("epsc", [128, 1], F32)
    hkw = sb("hkw", [128, 2, 2, 4, 32], BF16)
    posT = sb("posT", [64, 2, 32], BF16)
    arena0 = cur[0]

    vtok = [sb("vtok%d" % i, [128, D], F32) for i in range(2)]
    vhat = [sb("vhat%d" % i, [128, D], BF16) for i in range(2)]
    uT = [sb("uT%d" % c, [128, TT], BF16) for c in range(8)]
    oTg = [sb("oTg%d" % c, [128, TT], BF16) for c in range(8)]
    gbc = sb("gbc", [128, D], F32)
    bnst = sb("bnst", [128, 32], F32)
    wsT_l = sb("wsT_l", [128, 8, 128], BF16)
    bm_l = sb("bm_l", [128, 8, 128], F32)
    arena_end = cur[0]
    tilesA = vtok + vhat + uT + oTg + [gbc, bnst, wsT_l, bm_l]
    cur[0] = arena0
    qT_all = sb("qT_all", [128, 8, TT], BF16)
    gsT = sb("gsT", [48, TT], BF16)
    pT = [sb("pT%d" % i, [128, TT], BF16) for i in range(4)]
    o_acc = [sb("o_acc%d" % i, [64, TT], F32) for i in range(4)]
    o_tmp = [sb("o_tmp%d" % i, [64, TT], F32) for i in range(1)]
    rden = [sb("rden%d" % i, [64, TT], F32) for i in range(1)]
    ffac = [sb("ffac%d" % i, [64, TT], F32) for i in range(1)]
    oT_all = sb("oT_all", [128, 8, TT], BF16)
    impb = sb("impb", [128, 4, 64], F32)
    arena_end = max(arena_end, cur[0])
    tilesB = [qT_all] + [gsT] + pT + o_acc + o_tmp + rden + ffac + [oT_all] + [impb]
    cur[0] = arena0
    xio = [sb("xio%d" % s, [128, D], F32) for s in range(4)]
    stage = [sb("stage%d" % i, [128, 2048], F32) for i in range(2)]
    stage_sem = [P.new_sem("stg") for _ in range(2)]
    modT = sb("modT", [128, 5, 48, BPC], F32)
    adab = sb("adab", [128, 5, 48], F32)
    normg = sb("normg", [128, 17, 8], F32)
    wsT_p = sb("wsT_p", [128, 8, 128], BF16)
    bm_p = sb("bm_p", [128, 8, 128], F32)
    arena_end = max(arena_end, cur[0])
    tilesC = xio + stage + [modT, adab, normg, wsT_p, bm_p]
    cur[0] = arena0
    hid = [[sb("hid%d_%d" % (i, c), [128, TT], BF16) for c in range(4)] for i in range(2)]
    relu = [sb("relu%d" % i, [128, TT], F32) for i in range(2)]
    arena_end = max(arena_end, cur[0])
    tilesD = hid[0] + hid[1] + relu
    cur[0] = arena_end
    assert cur[0] <= top, ("SBUF overflow", cur[0], top)
    print("SBUF used", cur[0], "of", top)
    overlays = {"A": tilesA, "B": tilesB, "C": tilesC, "D": tilesD}

    def phase(name):
        old = []
        for k, v in overlays.items():
            if k != name:
                old.extend(t.d for t in v)
        P.handover(old, [t.d for t in overlays[name]])

    psb = [Buf(nc.alloc_psum_tensor("ps%d" % i, [128, 512], F32)) for i in range(8)]
    psi = [0]

    PS_GEN = [0, 1, 2, 3, 7]

    def ps():
        b = psb[PS_GEN[psi[0] % 5]]
        psi[0] += 1
        return b

    acc_i = [0]
    pT_rot = [0]
    pss_i = [0]
    psm_i = [0]

    def ps_s():
        b = psb[(0, 1, 2, 6)[pss_i[0] % 4]]
        pss_i[0] += 1
        return b

    def ps_m():
        b = psb[(3, 7)[psm_i[0] % 2]]
        psm_i[0] += 1
        return b

    def ps_acc():
        k = acc_i[0] % 2
        acc_i[0] += 1
        return psb[4 + k]


    def vaug(buf, blk, rows=128):
        return buf.t[0:rows, blk * 128:(blk + 1) * 128]

    def mmg(out_ap, pairs, reads, wr, first=True, last=True):
        n = len(pairs)

        def fn(e):
            r = None
            for i, (l, rr) in enumerate(pairs):
                r = e.matmul(out_ap, l, rr, start=(first and i == 0), stop=(last and i == n - 1))
            return r

        P.op("pe", fn, reads=reads, writes=[wr])

    def mm_multi(items, reads, wr):
        def fn(e):
            r = None
            for (o, l, rr) in items:
                r = e.matmul(o, l, rr, start=True, stop=True)
            return r

        P.op("pe", fn, reads=reads, writes=[wr])

    def act(out_ap, in_ap, func, reads, wr, bias=None, scale=None):
        kw = {}
        if bias is not None:
            kw["bias"] = bias
        if scale is not None:
            kw["scale"] = scale
        P.op("act", lambda e: e.activation(out=out_ap, in_=in_ap, func=func, **kw), reads=reads, writes=[wr])

    def tt(eng, out_ap, a, b, op, reads, wr):
        P.op(eng, lambda e: e.tensor_tensor(out=out_ap, in0=a, in1=b, op=op), reads=reads, writes=[wr])

    def stt(eng, out_ap, a, scalar, b, op0, op1, reads, wr):
        P.op(eng, lambda e: e.scalar_tensor_tensor(out=out_ap, in0=a, scalar=scalar, in1=b, op0=op0, op1=op1),
             reads=reads, writes=[wr])

    def ts(eng, out_ap, a, s1, s2, op0, op1, reads, wr):
        if op1 is None:
            P.op(eng, lambda e: e.tensor_scalar(out=out_ap, in0=a, scalar1=s1, scalar2=None, op0=op0),
                 reads=reads, writes=[wr])
        else:
            P.op(eng, lambda e: e.tensor_scalar(out=out_ap, in0=a, scalar1=s1, scalar2=s2, op0=op0, op1=op1),
                 reads=reads, writes=[wr])

    def cp(eng, out_ap, in_ap, reads, wr):
        P.op(eng, lambda e: e.tensor_copy(out=out_ap, in_=in_ap), reads=reads, writes=[wr])

    def dma(eng, out_ap, in_ap, sem, reads, wr):
        P.op(eng, lambda e: e.dma_start(out=out_ap, in_=in_ap), reads=reads, writes=[wr], dma_sem=sem)

    def recip(out_ap, in_ap, reads, wr):
        P.op("dve", lambda e: e.reciprocal(out=out_ap, in_=in_ap), reads=reads, writes=[wr])

    def transp(out_ap, in_ap, idn, reads, wr):
        P.op("pe", lambda e: e.transpose(out_ap, in_ap, idn), reads=reads, writes=[wr])

    def memset(eng, ap, val, wr):
        P.op(eng, lambda e: e.memset(ap, val), writes=[wr])

    _bsem = {}

    def bsem(buf):
        k = id(buf)
        if k not in _bsem:
            _bsem[k] = P.new_sem("b")
        return _bsem[k]

    def ldc(eng, buf, out_ap, src):
        dma(eng, out_ap, src, bsem(buf), [], buf.d)

    def load_x(b, i):
        t0 = i * TT
        for s in range(4):
            dma("pool", xio[s].t[:, :], x_d[b, t0 + s * 128:t0 + (s + 1) * 128, :], xio_sem[s], [], xio[s].d)

    ldc("sp", ident, ident.t[:, :], ident_d)
    ldc("sp", tri, tri.t[:, :], tri_d)
    ldc("pool", keepc, keepc.t[:, :, :], keep_d.rearrange("p (a b) -> p a b", b=32))
    ldc("pool", addcc, addcc.t[:, :, :], addc_d.rearrange("p (a b) -> p a b", b=32))
    ldc("pool", ident_bf, ident_bf.t[:, :], ident_d)
    ldc("pool", causal, causal.t[:, :], causal_d)
    ldc("pool", acausal, acausal.t[:, :], acausal_d)
    ldc("pool", cmpb, cmpb.t[:, :, :], cmpb_d.rearrange("p (a b) -> p a b", b=128))
    ldc("pool", Ec, Ec.t[:, :, :], E_d.rearrange("p (a b) -> p a b", b=128))
    ldc("pool", ovl, ovl.t[:, :], ovl_d)
    ldc("pool", selc, selc.t[:, :, :], sel_d.rearrange("p (a b) -> p a b", b=64))
    memset("dve", ones_bf.t[:, :], 1.0, ones_bf.d)
    for g in range(4):
        memset("dve", selbT[g].t[:, :], 0.0, selbT[g].d)
    memset("dve", epsc.t[:, :], EPS, epsc.d)
    memset("dve", vsel_all.t[:, :], 1.0, vones_d)
    memset("dve", vwin_all.t[:, :], 1.0, vones_d)
    memset("dve", v_cmp.t[:, :], 1.0, vones_d)
    for l in range(2):
        ldc("pool", wg, wg.t[:, l, :, :], b_w_g_d[l].rearrange("(k p) n -> p k n", p=128))
    memset("dve", w2k.t[:, :, :], 0.0, w2k.d)
    ldc("pool", w2k, w2k.t[:, :, 64:128], cmp_w2_d[0].rearrange("(k p) n -> p k n", p=128))
    ldc("pool", w2v, w2v.t[:, :, :], cmp_w2_d[1].rearrange("(k p) n -> p k n", p=128))
    ldc("sp", b2k, b2k.t[0:64, :], cmp_b2_d[0].rearrange("(p o) -> p o", o=1))
    ldc("sp", b2k, b2k.t[64:128, :], cmp_b2_d[0].rearrange("(p o) -> p o", o=1))
    ldc("pool", b2v, b2v.t[:, :], cmp_b2_d[1].rearrange("(o n) -> o n", o=1))
    ldc("sp", lnb, lnb.t[:, :], a_ln_b_d)
    ldc("pool", posT, posT.t[:, :, :], cmp_posT_d.rearrange("k d l -> d k l"))
    ldc("sp", posb, posb.t[:, :, :], cmp_b1_d.rearrange("p (k j) -> p k j", j=2))
    ldc("sp", small, small.t[:, 0:16], cT_d.rearrange("p k b -> p (k b)"))
    ldc("sp", adab, adab.t[:, 0:4, :], ada_b_d.rearrange("p (l c) -> p l c", c=48))
    ldc("sp", adab, adab.t[:, 4, 0:16], kv_ada_b_d)
    ldc("sp", normg, normg.t[:, 0:16, :], norm_g_d.rearrange("p (l c) -> p l c", c=8))
    ldc("sp", normg, normg.t[:, 16, :], kv_norm_g_d)

    load_x(0, 0)
    conv = {}

    def convert(key, src, dst, nelem):
        sem = P.new_sem("cv")
        tl = Tl()
        cols = nelem // 128
        s2 = src.rearrange("(p n) -> p n", p=128) if False else src
        step = 8192
        for c0 in range(0, cols, step):
            c1 = min(cols, c0 + step)
            dma("pool", dst[:, c0:c1], src[:, c0:c1], sem, [], tl)
        conv[key] = tl

    def flat2(ap, n):
        nd = len(ap.shape)
        names = " ".join("d%d" % i for i in range(nd))
        flat = ap.rearrange("%s -> (%s)" % (names, names))
        return flat.rearrange("(p n) -> p n", p=128)

    def conv_w(key, src_ap, dst_ap):
        n = 1
        for s in src_ap.shape:
            n *= s
        convert(key, flat2(src_ap, n), flat2(dst_ap, n), n)

    for l in range(2):
        conv_w(("a_in", l), a_w_in_d[l], s_a_w_in[l])
        conv_w(("a_out", l), a_w_out_d[l], s_a_w_out[l])
        conv_w(("ff_in", l), ff_w_in_d[l], s_ff_w_in[l])
        conv_w(("ff_out", l), ff_w_out_d[l], s_ff_w_out[l])
    conv_w(("kv",), kv_w_d, s_kv_w)
    conv_w(("w1",), cmp_w1_d, s_cmp_w1)
    for l in range(2, 4):
        conv_w(("b_q", l), b_w_q_d[l - 2], s_b_w_q[l - 2])
        conv_w(("b_out", l), b_w_out_d[l - 2], s_b_w_out[l - 2])
        conv_w(("ff_in", l), ff_w_in_d[l], s_ff_w_in[l])
        conv_w(("ff_out", l), ff_w_out_d[l], s_ff_w_out[l])

    act(small.t[:, 16:32], small.t[:, 0:16], AF.Silu, [small.d], small.d)
    cactv = small.t[:, 16:32].rearrange("p (k b) -> p k b", b=BPC)
    bi = 0
    phase("C")
    for l in range(5):
        nblk = 24 if l < 4 else 8
        src = (ada_w_d[l] if l < 4 else kv_ada_w_d)
        for blk in range(nblk):
            st = stage[bi % 2]
            ssem = stage_sem[bi % 2]
            bi += 1
            stv = st.t[:, :].rearrange("p (k n) -> p k n", n=256)
            dma("sp", stv, src[:, blk * 256:(blk + 1) * 256].rearrange("(k p) n -> p k n", p=128), ssem, [], st.d)
            for cc in range(2):
                pb = ps()
                mmg(pb.t[:, 0:BPC], [(stv[:, k, cc * 128:(cc + 1) * 128], cactv[:, k, :]) for k in range(8)],
                    [st.d, small.d], pb.d)
                ch = blk * 2 + cc
                ts("dve", modT.t[:, l, ch, :], pb.t[:, 0:BPC], adab.t[:, l, ch:ch + 1], None, ALU.add, None,
                   [pb.d, adab.d], modT.d)
    for b in range(BPC):
        for l in range(4):
            for (kind, sc_off, gi) in ((0, 8, 0), (3, 32, 2)):
                stt("dve", cst.t[:, b, l, kind, :], modT.t[:, l, sc_off:sc_off + 8, b], 1.0, normg.t[:, l * 4 + gi, :],
                    ALU.add, ALU.mult, [modT.d, normg.d], cst.d)
            for (kind, sh_off) in ((1, 0), (4, 24)):
                cp("dve", cst.t[:, b, l, kind, :], modT.t[:, l, sh_off:sh_off + 8, b], [modT.d], cst.d)
            for (kind, g_off, gi) in ((2, 16, 1), (5, 40, 3)):
                tt("dve", cst.t[:, b, l, kind, :], modT.t[:, l, g_off:g_off + 8, b], normg.t[:, l * 4 + gi, :], ALU.mult,
                   [modT.d, normg.d], cst.d)
        stt("dve", cstkv.t[:, b, 0, :], modT.t[:, 4, 8:16, b], 1.0, normg.t[:, 16, :], ALU.add, ALU.mult,
            [modT.d, normg.d], cstkv.d)
        cp("dve", cstkv.t[:, b, 1, :], modT.t[:, 4, 0:8, b], [modT.d], cstkv.d)

    for l in range(2):
        st = stage[bi % 2]
        ssem = stage_sem[bi % 2]
        bi += 1
        wv = st.t[:, 0:1024].rearrange("p (g s) -> p g s", s=128)
        dma("sp", wv, a_w_s_d[l].rearrange("g t s -> t g s"), ssem, [], st.d)
        tt("dve", wv, wv, tri.t[:, :].unsqueeze(1).to_broadcast([128, 8, 128]), ALU.mult, [st.d, tri.d], st.d)
        for half in range(2):
            pb = ps()
            for gg in range(4):
                g = half * 4 + gg
                transp(pb.t[:, gg * 128:(gg + 1) * 128], wv[:, g, :], ident.t[:, :], [st.d, ident.d], pb.d)
            cp("dve", wsT_p.t[:, half * 4:(half + 1) * 4, :], pb.t[:, :].rearrange("p (g t) -> p g t", t=128), [pb.d], wsT_p.d)
        ldc("sp", bm_p, bm_p.t[:, :, :], a_b_s_d[l].partition_broadcast(128).rearrange("p (g t) -> p g t", t=128))
        for half in range(2):
            pb = ps()
            mmg(pb.t[:, :], [(ones_bf.t[:, :], wsT_p.t[:, half * 4:(half + 1) * 4, :])], [ones_bf.d, wsT_p.d], pb.d)
            for gg in range(4):
                g = half * 4 + gg
                stt("dve", bm_p.t[:, g, :], pb.t[:, gg * 128:(gg + 1) * 128], lnb.t[:, l * 8 + g:l * 8 + g + 1],
                    bm_p.t[:, g, :], ALU.mult, ALU.add, [pb.d, lnb.d, bm_p.d], bm_p.d)
        dma("sp", s_wsT[l], wsT_p.t[:, :, :].rearrange("p g t -> p (g t)"), bsem(wsT_p), [wsT_p.d], scr_ws_d[l])
        dma("sp", s_bm[l], bm_p.t[:, :, :].rearrange("p g t -> p (g t)"), bsem(bm_p), [bm_p.d], scr_bm_d[l])
        wsT_p.d.addr(scr_ws_d[l].w)
        bm_p.d.addr(scr_bm_d[l].w)

    w1tl = conv[("w1",)]
    for kv in range(2):
        for jc in range(2):
            slot = wslot[(kv * 2 + jc) % NSLOT]
            sem = wsem[(kv * 2 + jc) % NSLOT]
            sv = slot.t[0:64, :].rearrange("p (l j) -> p l j", j=128)
            dma("sp", sv, s_cmp_w1[kv].rearrange("(l d) j -> d l j", d=64)[:, :, jc * 128:(jc + 1) * 128], sem, [w1tl], slot.d)
            pb = ps()
            mmg(pb.t[:, 0:1], [(sv[:, l, :], posT.t[:, kv, l:l + 1]) for l in range(32)], [slot.d, posT.d], pb.d)
            tt("dve", posb.t[:, kv, jc:jc + 1], pb.t[:, 0:1], posb.t[:, kv, jc:jc + 1], ALU.add, [pb.d, posb.d], posb.d)

    def wload(slot_i, out_ap, src_ap, dep):
        dma("sp", out_ap, src_ap, wsem[slot_i], [dep], wslot[slot_i].d)

    slot_rr = [0]

    def next_slot():
        i = slot_rr[0] % NSLOT
        slot_rr[0] += 1
        return i

    def rms_stats(src):
        pb = ps()
        for c in range(8):
            s = sq[c % 2]
            act(s.t[:, :], src[c].t[:, :], AF.Square, [src[c].d], s.d)
            mmg(pb.t[:, :], [(ones_bf.t[:, :], s.t[:, :])], [ones_bf.d, s.d], pb.d, first=(c == 0), last=(c == 7))
        act(rstd.t[:, :], pb.t[:, :], AF.Ln, [pb.d, epsc.d], rstd.d, bias=epsc.t[:, 0:1], scale=1.0 / D)
        act(rstd.t[:, :], rstd.t[:, :], AF.Exp, [rstd.d], rstd.d, scale=-0.5)

    def rmsmod(Gap, Sap):
        rms_stats(xT)
        for c in range(8):
            tf = tmpf[c % 2]
            stt("dve", tf.t[:, :], xT[c].t[:, :], Gap[:, c:c + 1], rstd.t[:, :], ALU.mult, ALU.mult,
                [xT[c].d, rstd.d, cst.d, cstkv.d], tf.d)
            act(hT[c].t[:, :], tf.t[:, :], AF.Identity, [tf.d, cst.d, cstkv.d], hT[c].d, bias=Sap[:, c:c + 1], scale=1.0)

    def residual(GGap):
        rms_stats(yT)
        for c in range(8):
            tf = tmpf[c % 2]
            stt("dve", tf.t[:, :], yT[c].t[:, :], GGap[:, c:c + 1], rstd.t[:, :], ALU.mult, ALU.mult,
                [yT[c].d, rstd.d, cst.d], tf.d)
            tt("pool", xT[c].t[:, :], xT[c].t[:, :], tf.t[:, :], ALU.add, [xT[c].d, tf.d], xT[c].d)

    def ffn(b, l):
        phase("D")
        rmsmod(cst.t[:, b, l, 3, :], cst.t[:, b, l, 4, :])
        tin = conv[("ff_in", l)]
        tout = conv[("ff_out", l)]
        hds = [h.d for h in hT]
        slots = {}

        def load(j):
            sa = next_slot()
            sbi = next_slot()
            A = wslot[sa].t[:, :].rearrange("p (k n) -> p k n", n=512)
            B = wslot[sbi].t[:, :].rearrange("p (k n) -> p k n", n=1024)
            wload(sa, A, s_ff_w_in[l][:, j * 512:(j + 1) * 512].rearrange("(k p) n -> p k n", p=128), tin)
            wload(sbi, B, s_ff_w_out[l][j * 512:(j + 1) * 512, :].rearrange("(k p) n -> p k n", p=128), tout)
            slots[j] = (sa, sbi, A, B)

        def hidden(j):
            sa, sbi, A, B = slots[j]
            hb = hid[j % 2]
            for c in range(4):
                pb = ps()
                mmg(pb.t[:, :], [(A[:, k, c * 128:(c + 1) * 128], hT[k].t[:, :]) for k in range(8)],
                    [wslot[sa].d] + hds, pb.d)
                r = relu[c % 2]
                act(r.t[:, :], pb.t[:, :], AF.Relu, [pb.d], r.d)
                tt("pool", hb[c].t[:, :], r.t[:, :], r.t[:, :], ALU.mult, [r.d], hb[c].d)

        def ypart(j):
            sa, sbi, A, B = slots[j]
            hb = hid[j % 2]
            for n in range(8):
                pb = ps()
                mmg(pb.t[:, :], [(B[:, c, n * 128:(n + 1) * 128], hb[c].t[:, :]) for c in range(4)],
                    [wslot[sbi].d] + [h.d for h in hb], pb.d)
                if j == 0:
                    act(yT[n].t[:, :], pb.t[:, :], AF.Copy, [pb.d], yT[n].d)
                else:
                    tt("dve", yT[n].t[:, :], pb.t[:, :], yT[n].t[:, :], ALU.add, [pb.d, yT[n].d], yT[n].d)

        load(0)
        hidden(0)
        for j in range(8):
            if j + 1 < 8:
                load(j + 1)
                hidden(j + 1)
            ypart(j)
        residual(cst.t[:, b, l, 5, :])

    def gmlp(b, l):
        phase("A")
        rmsmod(cst.t[:, b, l, 0, :], cst.t[:, b, l, 1, :])
        tin = conv[("a_in", l)]
        hds = [h.d for h in hT]
        ldc("sp", gbc, gbc.t[:, :], a_ln_g_d[l].partition_broadcast(128))
        dma("sp", wsT_l.t[:, :, :].rearrange("p g t -> p (g t)"), s_wsT[l], bsem(wsT_l), [scr_ws_d[l]], wsT_l.d)
        dma("sp", bm_l.t[:, :, :].rearrange("p g t -> p (g t)"), s_bm[l], bsem(bm_l), [scr_bm_d[l]], bm_l.d)
        for half in range(2):
            si = next_slot()
            W = wslot[si].t[:, :].rearrange("p (k n) -> p k n", n=512)
            wload(si, W, s_a_w_in[l][:, half * 512:(half + 1) * 512].rearrange("(k p) n -> p k n", p=128), tin)
            for cc in range(4):
                n = half * 4 + cc
                pb = ps()
                mmg(pb.t[:, :], [(W[:, k, cc * 128:(cc + 1) * 128], hT[k].t[:, :]) for k in range(8)],
                    [wslot[si].d] + hds, pb.d)
                act(uT[n].t[:, :], pb.t[:, :], AF.Gelu_apprx_tanh, [pb.d], uT[n].d)
        sv = [next_slot(), next_slot()]
        Wv = []
        for half in range(2):
            W = wslot[sv[half]].t[:, :].rearrange("p (k n) -> p k n", n=512)
            wload(sv[half], W, s_a_w_in[l][:, D + half * 512:D + (half + 1) * 512].rearrange("(k p) n -> p k n", p=128), tin)
            Wv.append(W)
        def vmat(s):
            vt = vtok[s % 2]
            for half in range(2):
                pb = ps()
                mmg(pb.t[:, :], [(hT[k].t[:, s * 128:(s + 1) * 128], Wv[half][:, k, :]) for k in range(8)],
                    [wslot[sv[half]].d] + hds, pb.d)
                act(vt.t[:, half * 512:(half + 1) * 512], pb.t[:, :], AF.Gelu_apprx_tanh, [pb.d], vt.d)

        def vrest(s):
            vt = vtok[s % 2]
            vh = vhat[s % 2]
            for half in range(2):
                P.op("dve", lambda e, o=bnst.t[:, half * 6:half * 6 + 6], i=vt.t[:, half * 512:(half + 1) * 512]: e.bn_stats(out=o, in_=i),
                     reads=[vt.d], writes=[bnst.d])
            P.op("dve", lambda e, o=bnst.t[:, 16:18], i=bnst.t[:, 0:12].rearrange("p (a b) -> p a b", b=6): e.bn_aggr(out=o, in_=i),
                 reads=[bnst.d], writes=[bnst.d])
            act(bnst.t[:, 18:19], bnst.t[:, 17:18], AF.Sqrt, [bnst.d, epsc.d], bnst.d, bias=epsc.t[:, 0:1], scale=1.0)
            recip(bnst.t[:, 19:20], bnst.t[:, 18:19], [bnst.d], bnst.d)
            stt("dve", vt.t[:, :], vt.t[:, :], bnst.t[:, 16:17], gbc.t[:, :], ALU.subtract, ALU.mult,
                [vt.d, bnst.d, gbc.d], vt.d)
            act(vh.t[:, :], vt.t[:, :], AF.Identity, [vt.d, bnst.d], vh.d, scale=bnst.t[:, 19:20])
            for half in range(2):
                pb = ps()
                mm_multi([(pb.t[:, gg * 128:(gg + 1) * 128], vh.t[:, (half * 4 + gg) * 128:(half * 4 + gg + 1) * 128],
                           wsT_l.t[:, half * 4 + gg, :]) for gg in range(4)], [vh.d, wsT_l.d], pb.d)
                tf = tmpf[half]
                tt("dve", tf.t[:, :], pb.t[:, :], bm_l.t[:, half * 4:(half + 1) * 4, :].rearrange("p g t -> p (g t)"),
                   ALU.add, [pb.d, bm_l.d], tf.d)
                for gg in range(4):
                    g = half * 4 + gg
                    tt("pool", oTg[g].t[:, s * 128:(s + 1) * 128], tf.t[:, gg * 128:(gg + 1) * 128],
                       uT[g].t[:, s * 128:(s + 1) * 128], ALU.mult, [tf.d, uT[g].d], oTg[g].d)

        vmat(0)
        for s in range(4):
            if s + 1 < 4:
                vmat(s + 1)
            vrest(s)
        tout = conv[("a_out", l)]
        ods = [o.d for o in oTg]
        for half in range(2):
            si = next_slot()
            W = wslot[si].t[:, :].rearrange("p (k n) -> p k n", n=512)
            wload(si, W, s_a_w_out[l][:, half * 512:(half + 1) * 512].rearrange("(k p) n -> p k n", p=128), tout)
            for cc in range(4):
                n = half * 4 + cc
                pb = ps()
                mmg(pb.t[:, :], [(W[:, k, cc * 128:(cc + 1) * 128], oTg[k].t[:, :]) for k in range(8)],
                    [wslot[si].d] + ods, pb.d)
                act(yT[n].t[:, :], pb.t[:, :], AF.Copy, [pb.d], yT[n].d)
        residual(cst.t[:, b, l, 2, :])

    def kvproj(b, i):
        rmsmod(cstkv.t[:, b, 0, :], cstkv.t[:, b, 1, :])
        tkv = conv[("kv",)]
        hds = [h.d for h in hT]
        t0 = i * TT
        for blk in range(3):
            si = next_slot()
            W = wslot[si].t[:, :].rearrange("p (k n) -> p k n", n=512)
            wload(si, W, s_kv_w[:, blk * 512:(blk + 1) * 512].rearrange("(k p) n -> p k n", p=128), tkv)
            for gh in range(2):
                pb = ps()
                mmg(pb.t[:, :], [(W[:, k, gh * 128:(gh + 1) * 128], hT[k].t[:, :]) for k in range(8)], [wslot[si].d] + hds, pb.d)
                if blk == 0:
                    act(raw_k.t[:, gh, 16:16 + TT], pb.t[:, :], AF.Copy, [pb.d], raw_k.d)
                elif blk == 1:
                    act(k_selT.t[:, gh, t0:t0 + TT], pb.t[:, :], AF.Copy, [pb.d], k_selT.d)
                else:
                    act(k_winT.t[:, gh, t0:t0 + TT], pb.t[:, :], AF.Copy, [pb.d], k_winT.d)
            if blk == 0:
                for gh in range(2):
                    pb = ps()
                    mmg(pb.t[:, :], [(W[:, k, 256 + gh * 128:256 + (gh + 1) * 128], hT[k].t[:, :]) for k in range(8)],
                        [wslot[si].d] + hds, pb.d)
                    act(raw_v.t[:, gh, 16:16 + TT], pb.t[:, :], AF.Copy, [pb.d], raw_v.d)
            else:
                vv, vd = (vsel_all, vsel_d) if blk == 1 else (vwin_all, vwin_d)
                for s in range(4):
                    kt = i * 4 + s
                    pb = ps()
                    mmg(pb.t[:, 0:256], [(hT[k].t[:, s * 128:(s + 1) * 128], W[:, k, 256:512]) for k in range(8)],
                        [wslot[si].d] + hds, pb.d)
                    cp("dve", vv.t[:, kt * 512:(kt + 1) * 512].rearrange("p (g c) -> p g c", c=128)[:, :, 0:64],
                       pb.t[:, 0:256].rearrange("p (g d) -> p g d", d=64), [pb.d, vones_d], vd[kt])
        for kv in range(2):
            raw = raw_k if kv == 0 else raw_v
            for jc in range(2):
                si = next_slot()
                sv = wslot[si].t[:, :].rearrange("p (l j) -> p l j", j=128)
                src = s_cmp_w1[kv].rearrange("(l d) j -> d l j", d=64)[:, :, jc * 128:(jc + 1) * 128]
                wload(si, sv[0:64], src, conv[("w1",)])
                wload(si, sv[64:128], src, conv[("w1",)])
                for ph in range(2):
                    pb = ps()
                    mmg(pb.t[:, 0:64].rearrange("p (a m) -> p a m", m=32),
                        [(sv[ph * 64:(ph + 1) * 64, l, :], raw.t[ph * 64:(ph + 1) * 64, :, l:l + 16 * 31 + 1:16]) for l in range(32)],
                        [wslot[si].d, raw.d], pb.d)
                    for gh in range(2):
                        act(hkw.t[:, jc, kv, gh * 2 + ph, :], pb.t[:, gh * 32:(gh + 1) * 32], AF.Gelu_apprx_tanh,
                            [pb.d, posb.d], hkw.d, bias=posb.t[:, kv, jc:jc + 1], scale=1.0)
        for gh in range(2):
            pb = ps()
            pairs = []
            for ph in range(2):
                for jc in range(2):
                    lw = w2k.t[:, jc, 64:192] if ph == 0 else w2k.t[:, jc, 0:128]
                    pairs.append((lw, hkw.t[:, jc, 0, gh * 2 + ph, :]))
            mmg(pb.t[:, 0:32], pairs, [w2k.d, hkw.d], pb.d)
            act(k_cmpT.t[:, gh, i * 32:(i + 1) * 32], pb.t[:, 0:32], AF.Identity, [pb.d, b2k.d], k_cmpT.d,
                bias=b2k.t[:, 0:1], scale=1.0)
        pb = ps()
        for g in range(4):
            pairs = [(hkw.t[:, jc, 1, g, :], w2v.t[:, jc, :]) for jc in range(2)]
            pairs.append((ones_bf.t[0:1, 0:32], b2v.t[0:1, :]))
            mmg(pb.t[0:32, g * 64:(g + 1) * 64], pairs, [hkw.d, w2v.d, b2v.d, ones_bf.d], pb.d)
        cp("dve", v_cmp.t[i * 32:(i + 1) * 32, :].rearrange("p (g c) -> p g c", c=128)[:, :, 0:64],
           pb.t[0:32, 0:256].rearrange("p (g d) -> p g d", d=64), [pb.d, vones_d], v_cmp.d)
        cp("pool", raw_k.t[:, :, 0:16], raw_k.t[:, :, TT:TT + 16], [raw_k.d], raw_k.d)
        cp("pool", raw_v.t[:, :, 0:16], raw_v.t[:, :, TT:TT + 16], [raw_v.d], raw_v.d)

    def branch_finish(psa, g, br, s, oa, first, guard, final=False):
        rd = rden[0]
        ff = ffac[0]
        ot = o_tmp[0]
        if guard:
            ts("dve", rd.t[:, :], psa.t[64:128, :], 1e-30, None, ALU.max, None, [psa.d], rd.d)
            act(rd.t[:, :], rd.t[:, :], AF.Ln, [rd.d], rd.d)
        else:
            act(rd.t[:, :], psa.t[64:128, :], AF.Ln, [psa.d], rd.d)
        act(rd.t[:, :], rd.t[:, :], AF.Exp, [rd.d], rd.d, scale=-1.0)
        pg = ps_m()
        mm_multi([(pg.t[0:64, hh * 128:(hh + 1) * 128], selc.t[:, (g * 4 + hh) * 3 + br, :], gsT.t[:, s * 128:(s + 1) * 128])
                  for hh in range(4)], [selc.d, gsT.d], pg.d)
        tt("dve", ff.t[:, :], pg.t[0:64, :], rd.t[:, :], ALU.mult, [pg.d, rd.d], ff.d)
        if first:
            tt("dve", oa.t[:, :], psa.t[0:64, :], ff.t[:, :], ALU.mult, [psa.d, ff.d], oa.d)
        else:
            tt("dve", ot.t[:, :], psa.t[0:64, :], ff.t[:, :], ALU.mult, [psa.d, ff.d], ot.d)
            if final:
                oav = oa.t[:, :].rearrange("p (j two t) -> p j two t", two=2, t=128)
                otv = ot.t[:, :].rearrange("p (j two t) -> p j two t", two=2, t=128)
                for half in range(2):
                    tt("pool" if half == 0 else "dve",
                       oT_all.t[half * 64:(half + 1) * 64, g * 2:g * 2 + 2, s * 128:(s + 1) * 128],
                       oav[:, :, half, :], otv[:, :, half, :], ALU.add, [oa.d, ot.d], oT_all.d)
            else:
                tt("pool", oa.t[:, :], oa.t[:, :], ot.t[:, :], ALU.add, [oa.d, ot.d], oa.d)

    def nsa(b, l, i):
        phase("B")
        j = l - 2
        rmsmod(cst.t[:, b, l, 0, :], cst.t[:, b, l, 1, :])
        hds = [h.d for h in hT]
        tq = conv[("b_q", l)]
        for half in range(2):
            si = next_slot()
            W = wslot[si].t[:, :].rearrange("p (k n) -> p k n", n=512)
            wload(si, W, s_b_w_q[j][:, half * 512:(half + 1) * 512].rearrange("(k p) n -> p k n", p=128), tq)
            for cc in range(4):
                n = half * 4 + cc
                pb = ps()
                mmg(pb.t[:, :], [(W[:, k, cc * 128:(cc + 1) * 128], hT[k].t[:, :]) for k in range(8)], [wslot[si].d] + hds, pb.d)
                act(qT_all.t[:, n, :], pb.t[:, :], AF.Identity, [pb.d], qT_all.d, scale=0.125)
        pb = ps()
        mmg(pb.t[0:48, :], [(wg.t[:, j, k, :], hT[k].t[:, :]) for k in range(8)], [wg.d] + hds, pb.d)
        act(gsT.t[:, :], pb.t[0:48, :], AF.Sigmoid, [pb.d], gsT.d)
        ncc = 32 * (i + 1)
        b4 = lambda ap: ap.unsqueeze(1).to_broadcast([ap.shape[0], 4, 128])

        def make_scores(pb, rows, lhsT, rhs4, extra):
            n = len(extra)
            full = pb.t[0:rows, :].rearrange("p (a t) -> p a t", t=128)
            ex = list(extra)

            def fn(e):
                r = e.matmul(full, lhsT, rhs4, start=True, stop=(n == 0), skip_group_check=True)
                for ei, (el, er) in enumerate(ex):
                    r = e.matmul(full, el, er, start=False, stop=(ei == n - 1), skip_group_check=True)
                return r
            return fn

        units = []
        for s in range(4):
            qt = i * 4 + s
            use_sel = qt >= 8
            kt0 = max(0, qt - 4)
            for g in range(4):
                units.append(("cmp", s, g, 0, True, True))
                for kt in range(kt0, qt + 1):
                    units.append(("win", s, g, kt, kt == kt0, kt == qt))
            for g in range(4):
                for kt in range(qt + 1):
                    units.append(("sel", s, g, kt, kt == 0, kt == qt))
        state = {}
        accs = {}
        deferred = []

        def emit_scores(u):
            kind, s, g, kt, ufirst, ulast = u
            qt = i * 4 + s
            gh, ph = g // 2, g % 2
            pr = slice(ph * 64, (ph + 1) * 64)
            qds = [qT_all.d]
            rhss = qT_all.t[pr, gh * 4:(gh + 1) * 4, s * 128:(s + 1) * 128]
            pb = ps_s()
            if kind == "cmp":
                P.op("pe", make_scores(pb, ncc, k_cmpT.t[pr, gh, 0:ncc], rhss, [(ident_bf.t[:, 0:ncc], b4(cmpb.t[:, qt, :]))]),
                     reads=qds + [k_cmpT.d, ident_bf.d, cmpb.d], writes=[pb.d])
            elif kind == "sel":
                extra = []
                rd = list(qds) + [k_selT.d]
                if qt >= 8:
                    extra.append((Ec.t[:, kt, :], b4(selbT[g].t[:, :])))
                    rd += [Ec.d, selbT[g].d]
                if kt == qt:
                    extra.append((ident_bf.t[:, :], b4(causal.t[:, :])))
                    rd += [ident_bf.d, causal.d]
                P.op("pe", make_scores(pb, 128, k_selT.t[pr, gh, kt * 128:(kt + 1) * 128], rhss, extra), reads=rd, writes=[pb.d])
            else:
                extra = []
                rd = list(qds) + [k_winT.d]
                if kt == qt:
                    extra.append((ident_bf.t[:, :], b4(causal.t[:, :])))
                    rd += [ident_bf.d, causal.d]
                if kt == qt - 4:
                    extra.append((ident_bf.t[:, :], b4(acausal.t[:, :])))
                    rd += [ident_bf.d, acausal.d]
                P.op("pe", make_scores(pb, 128, k_winT.t[pr, gh, kt * 128:(kt + 1) * 128], rhss, extra), reads=rd, writes=[pb.d])
            state[u] = pb

        def emit_post(u):
            kind, s, g, kt, ufirst, ulast = u
            qt = i * 4 + s
            pb = state.pop(u)
            oa = o_acc[g]
            rows = ncc if kind == "cmp" else 128
            p = pT[psi[0] % 3]
            psi[0] += 0
            pT_rot[0] += 1
            p = pT[pT_rot[0] % 4]
            act(p.t[0:rows, :], pb.t[0:rows, :], AF.Exp, [pb.d], p.d)
            key = (kind, s, g)
            if ufirst:
                accs[key] = ps_acc()
            psa = accs[key]
            if kind == "cmp":
                lw, lwd = vaug(v_cmp, g, rows), [v_cmp.d, vones_d]
            elif kind == "sel":
                lw, lwd = vaug(vsel_all, kt * 4 + g), [vsel_d[kt], vones_d]
            else:
                lw, lwd = vaug(vwin_all, kt * 4 + g), [vwin_d[kt], vones_d]
            mmg(psa.t[:, :], [(lw, p.t[0:rows, :])], lwd + [p.d], psa.d, first=ufirst, last=ulast)
            if not ulast:
                return
            del accs[key]
            if kind == "cmp":
                use_sel = qt >= 8
                if use_sel:
                    pim = ps_m()
                    mm_multi([(pim.t[:, hh * 64:hh * 64 + 33], p.t[0:ncc, hh * 128:(hh + 1) * 128], ovl.t[0:ncc, :]) for hh in range(4)],
                             [p.d, ovl.d], pim.d)
                branch_finish(psa, g, 0, s, oa, True, qt == 0)
                if use_sel:
                    pv = pim.t[:, 0:256].rearrange("p (h c) -> p h c", c=64)
                    recip(impb.t[:, 0, 32:36], pv[:, :, 32], [pim.d], impb.d)
                    ts("dve", impb.t[:, 1, 0:32], pv[:, 0, 0:32], impb.t[:, 0, 32:33], None, ALU.mult, None, [pim.d, impb.d], impb.d)
                    for hh in range(1, 4):
                        stt("dve", impb.t[:, 1, 0:32], pv[:, hh, 0:32], impb.t[:, 0, 32 + hh:33 + hh], impb.t[:, 1, 0:32],
                            ALU.mult, ALU.add, [pim.d, impb.d], impb.d)
                    tt("dve", impb.t[:, 1, 0:32], impb.t[:, 1, 0:32], keepc.t[:, qt, :], ALU.mult, [impb.d, keepc.d], impb.d)
                    tt("dve", impb.t[:, 1, 0:32], impb.t[:, 1, 0:32], addcc.t[:, qt, :], ALU.add, [impb.d, addcc.d], impb.d)
                    P.op("dve", lambda e: e.max(out=impb.t[:, 2, 0:8], in_=impb.t[:, 1, 0:32]), reads=[impb.d], writes=[impb.d])
                    P.op("dve", lambda e: e.match_replace(out=impb.t[:, 3, 0:32], in_to_replace=impb.t[:, 2, 0:8],
                                                          in_values=impb.t[:, 1, 0:32], imm_value=-2.0), reads=[impb.d], writes=[impb.d])
                    P.op("dve", lambda e: e.max(out=impb.t[:, 2, 8:16], in_=impb.t[:, 3, 0:32]), reads=[impb.d], writes=[impb.d])
                    sb_src = selb_f[g]
                    ts("dve", sb_src.t[:, :], impb.t[:, 1, 0:32], impb.t[:, 2, 15:16], NEG, ALU.is_lt, ALU.mult, [impb.d], sb_src.d)

                    def fin(g=g, sb_src=sb_src):
                        ptr = ps_m()
                        transp(ptr.t[0:32, 0:128], sb_src.t[:, :], ident.t[:, :], [sb_src.d, ident.d], ptr.d)
                        cp("dve", selbT[g].t[0:32, :], ptr.t[0:32, 0:128], [ptr.d], selbT[g].d)
                    deferred.append([4, (s, g), fin])
            elif kind == "win":
                branch_finish(psa, g, 2, s, oa, False, False)
            else:
                branch_finish(psa, g, 1, s, oa, False, False, final=True)

        def run_deferred(force_key=None):
            keep = []
            for item in deferred:
                if item[0] <= 0 or (force_key is not None and item[1] == force_key):
                    item[2]()
                else:
                    keep.append(item)
            deferred[:] = keep

        def scores_checked(u):
            if u[0] == "sel" and u[3] == 0:
                run_deferred(force_key=(u[1], u[2]))
            emit_scores(u)

        LOOK = 3
        for idx in range(min(LOOK, len(units))):
            scores_checked(units[idx])
        for idx in range(len(units)):
            if idx + LOOK < len(units):
                scores_checked(units[idx + LOOK])
            for item in deferred:
                item[0] -= 1
            run_deferred()
            emit_post(units[idx])
        run_deferred()
        for item in list(deferred):
            item[2]()
        deferred[:] = []
        tout = conv[("b_out", l)]
        ods = [oT_all.d]
        for half in range(2):
            si = next_slot()
            W = wslot[si].t[:, :].rearrange("p (k n) -> p k n", n=512)
            wload(si, W, s_b_w_out[j][:, half * 512:(half + 1) * 512].rearrange("(k p) n -> p k n", p=128), tout)
            for cc in range(4):
                n = half * 4 + cc
                pb = ps()
                mmg(pb.t[:, :], [(W[:, k, cc * 128:(cc + 1) * 128], oT_all.t[:, k, :]) for k in range(8)], [wslot[si].d] + ods, pb.d)
                act(yT[n].t[:, :], pb.t[:, :], AF.Copy, [pb.d], yT[n].d)
        residual(cst.t[:, b, l, 2, :])

    nst = [0]
    for b in range(BPC):
        memset("pool", raw_k.t[:, :, 0:16], 0.0, raw_k.d)
        memset("pool", raw_v.t[:, :, 0:16], 0.0, raw_v.d)
        for i in range(NTILE):
            t0 = i * TT
            phase("C")
            if not (b == 0 and i == 0):
                load_x(b, i)
            for c in range(8):
                pb = ps()
                for s in range(4):
                    transp(pb.t[:, s * 128:(s + 1) * 128], xio[s].t[:, c * 128:(c + 1) * 128], ident.t[:, :], [xio[s].d, ident.d], pb.d)
                act(xT[c].t[:, :], pb.t[:, :], AF.Copy, [pb.d], xT[c].d)
            for l in range(min(2, n_layers)):
                gmlp(b, l)
                ffn(b, l)
            if n_layers > 2:
                kvproj(b, i)
                for l in range(2, n_layers):
                    nsa(b, l, i)
                    ffn(b, l)
            phase("C")
            for s in range(4):
                for half in range(2):
                    pb = ps()
                    for cc in range(4):
                        c = half * 4 + cc
                        transp(pb.t[:, cc * 128:(cc + 1) * 128], xT[c].t[:, s * 128:(s + 1) * 128], ident.t[:, :], [xT[c].d, ident.d], pb.d)
                    act(xio[s].t[:, half * 512:(half + 1) * 512], pb.t[:, :], AF.Copy, [pb.d], xio[s].d)
                dma("pool", out_d[b, t0 + s * 128:t0 + (s + 1) * 128, :], xio[s].t[:, :], xio_sem[s], [xio[s].d], xio[s].d)
                nst[0] += 1
    finals = []
    for s in range(4):
        finals.append((xio_sem[s], P.dcnt[id(xio_sem[s])]))
    print("ops:", {e: len(v) for e, v in P.ops.items()})
    P.emit(finals)
    return nc


def _consts():
    c = {}
    c["c_ident"] = np.eye(128, dtype=np.float32)
    tl = np.arange(128)
    c["c_tri"] = (tl[None, :] <= tl[:, None]).astype(np.float32)
    c["c_causal"] = np.where(tl[:, None] <= tl[None, :], 0.0, NEG).astype(np.float32)
    c["c_acausal"] = np.where(tl[:, None] > tl[None, :], 0.0, NEG).astype(np.float32)
    cc = np.arange(128)[:, None, None]
    qt = np.arange(16)[None, :, None]
    t = qt * 128 + tl[None, None, :]
    valid = (cc >= 1) & (16 * (cc - 1) + 31 <= t)
    c["c_cmpb"] = np.where(valid, 0.0, NEG).astype(np.float32).reshape(128, 16 * 128)
    j = np.arange(32)[:, None, None]
    key = np.arange(16)[None, :, None] * 128 + tl[None, None, :]
    Ef = np.zeros((128, 16 * 128), np.float32)
    Ef[0:32] = ((key // 64) == j).astype(np.float32).reshape(32, 16 * 128)
    c["c_E"] = Ef
    tq = np.arange(16)[None, :, None] * 128 + tl[:, None, None]
    curb = tq // 64
    jj = np.arange(32)[None, None, :]
    future = jj > curb
    forced = ((jj == 0) | (jj == curb) | (jj == curb - 1)) & (~future)
    keep = (~future) & (~forced)
    addc = np.where(forced, 1e4, np.where(future, -1.0, 0.0))
    c["c_keep"] = keep.astype(np.float32).reshape(128, 16 * 32)
    c["c_addc"] = addc.astype(np.float32).reshape(128, 16 * 32)
    ci = (np.arange(128) - 1)[:, None] * 16
    sj = np.arange(32)[None, :] * 64
    ov = ((ci < sj + 64) & (ci + 32 > sj)).astype(np.float32)
    ov[0, :] = 0.0
    c["c_ovl"] = np.concatenate([ov, np.ones((128, 1), np.float32)], axis=1)
    sel = np.zeros((48, 48, 64), np.float32)
    for k in range(48):
        sel[k, k, :] = 1.0
    c["c_sel"] = sel.reshape(48, 48 * 64)
    return c


def _prep_shared(inp):
    f = lambda a: np.ascontiguousarray(np.asarray(a, dtype=np.float32))
    sh = {}
    sh["ada_w"] = f(inp["ada_w"])
    sh["ada_bT"] = f(np.asarray(inp["ada_b"]).reshape(4, 48, 128).transpose(2, 0, 1).reshape(128, 4 * 48))
    sh["norm_gT"] = f(np.asarray(inp["norm_g"]).reshape(16, 8, 128).transpose(2, 0, 1).reshape(128, 16 * 8))
    sh["a_w_in"] = f(inp["a_w_in"])
    sh["a_ln_g"] = f(inp["a_ln_g"])
    sh["a_ln_bT"] = f(np.asarray(inp["a_ln_b"]).reshape(2, 8, 128).transpose(2, 0, 1).reshape(128, 16))
    sh["a_w_s"] = f(inp["a_w_s"])
    sh["a_b_s"] = f(np.asarray(inp["a_b_s"]).reshape(2, 8 * 128))
    sh["a_w_out"] = f(inp["a_w_out"])
    sh["kv_ada_w"] = f(inp["kv_ada_w"])
    sh["kv_ada_bT"] = f(np.asarray(inp["kv_ada_b"]).reshape(16, 128).T)
    sh["kv_norm_gT"] = f(np.asarray(inp["kv_norm_g"]).reshape(8, 128).T)
    sh["kv_w"] = f(inp["kv_w"])
    sh["cmp_posT"] = f(np.asarray(inp["cmp_pos"]).transpose(0, 2, 1))
    sh["cmp_w1"] = f(inp["cmp_w1"])
    sh["cmp_b1T"] = f(np.asarray(inp["cmp_b1"]).reshape(2, 2, 128).transpose(2, 0, 1).reshape(128, 4))
    sh["cmp_w2"] = f(inp["cmp_w2"])
    sh["cmp_b2"] = f(inp["cmp_b2"])
    bw = np.asarray(inp["b_w_in"], dtype=np.float32)
    q = bw[:, :, :1024].reshape(2, 1024, 2, 2, 4, 64)
    q = q.transpose(0, 1, 2, 4, 3, 5).reshape(2, 1024, 1024)
    sh["b_w_q"] = f(q)
    sh["b_w_g"] = f(bw[:, :, 1024:])
    sh["b_w_out"] = f(inp["b_w_out"])
    sh["ff_w_in"] = f(inp["ff_w_in"])
    sh["ff_w_out"] = f(inp["ff_w_out"])
    sh.update(_consts())
    return sh


_CACHE = {}


N_LAYERS = 4


def kernel(**inputs):
    n_layers = N_LAYERS
    if "nc" not in _CACHE:
        _CACHE["nc"] = build_program(n_layers)
    nc = _CACHE["nc"]
    sh = _prep_shared(inputs)
    x = np.asarray(inputs["x"], dtype=np.float32)
    c = np.asarray(inputs["c"], dtype=np.float32)
    in_maps = []
    for core in range(NCORES):
        m = dict(sh)
        m["x"] = np.ascontiguousarray(x[core * BPC:(core + 1) * BPC])
        cc = c[core * BPC:(core + 1) * BPC]
        m["cT"] = np.ascontiguousarray(cc.reshape(BPC, 8, 128).transpose(2, 1, 0))
        in_maps.append(m)
    res = run_bass_kernel_spmd(nc, in_maps, core_ids=list(range(NCORES)))
    out = np.concatenate([r["out"] for r in res.results], axis=0)
    return out.astype(np.float32)
```
